# Optimizing a Trainium2 kernel written in Bass

```python
import jax, jax.numpy as jnp
from jax import lax
import numpy as np

D_MODEL = 1024
BATCH = 16
SEQ = 2048
DEPTH = 1

N_MEM = 256
EPS = 1e-6
NEG_INF = -1e30
D_FF = 2816
N_HEADS_A = 8
D_QK = 64
D_V = 64
Q_LORA = 256
KV_LORA = 256
N_IDX = 4
D_IDX = 64
TOPK_MAX = 256
Q_BLOCK = 128
CONV_CH = 512
CONV_WIDTH = 31
N_HEADS_M = 4
D_HEAD_M = 128
N_BRANCH = 3
SPLITS = (Q_LORA, KV_LORA, D_IDX, N_IDX, 2 * CONV_CH, N_HEADS_M * D_HEAD_M, N_BRANCH * D_MODEL)
D_IN = sum(SPLITS)

kernel_name = "hybrid_dsa_conformer_memory_block"


def rms_norm(x, g):
    xf = x.astype(jnp.float32)
    y = xf * lax.rsqrt(jnp.mean(xf * xf, axis=-1, keepdims=True) + EPS)
    return (y * g.astype(jnp.float32)).astype(x.dtype)


def layer_norm(x, g, b):
    xf = x.astype(jnp.float32)
    mu = jnp.mean(xf, axis=-1, keepdims=True)
    xc = xf - mu
    var = jnp.mean(xc * xc, axis=-1, keepdims=True)
    return (xc * lax.rsqrt(var + EPS) * g.astype(jnp.float32) + b.astype(jnp.float32)).astype(x.dtype)


def half_step_ffn(x, g_pre, g_post, w_gu, w_down):
    gate, up = jnp.split(rms_norm(x, g_pre) @ w_gu, 2, axis=-1)
    return x + 0.5 * rms_norm((jax.nn.silu(gate) * up) @ w_down, g_post)


def dsa_attention(h, c_q, c_kv, k_idx_raw, w_idx_raw, q_norm_g, kv_norm_g, w_uq, w_uk, w_uv,
                  w_idx_q, idx_ln_g, idx_ln_b):
    B, T, _ = h.shape
    topk = min(TOPK_MAX, T // 4)
    cq = rms_norm(c_q, q_norm_g)
    ckv = rms_norm(c_kv, kv_norm_g)
    q = (cq @ w_uq).reshape(B, T, N_HEADS_A, D_QK)
    q_lat = jnp.einsum('bthd,chd->bthc', q, w_uk) * (D_QK ** -0.5)
    q_idx = ((cq @ w_idx_q) * (D_IDX ** -0.5)).reshape(B, T, N_IDX, D_IDX).astype(jnp.float32)
    k_idx = layer_norm(k_idx_raw, idx_ln_g, idx_ln_b).astype(jnp.float32)
    w_idx = (w_idx_raw * (N_IDX ** -0.5)).astype(jnp.float32)
    key_pos = jnp.arange(T)

    def block(start):
        qi = lax.dynamic_slice_in_dim(q_idx, start, Q_BLOCK, axis=1)
        wi = lax.dynamic_slice_in_dim(w_idx, start, Q_BLOCK, axis=1)
        ql = lax.dynamic_slice_in_dim(q_lat, start, Q_BLOCK, axis=1)
        qpos = start + jnp.arange(Q_BLOCK)
        logits = jnp.einsum('bqhd,bsd->bqhs', qi, k_idx)
        score = jnp.einsum('bqh,bqhs->bqs', wi, jax.nn.relu(logits))
        causal = key_pos[None, :] <= qpos[:, None]
        score = jnp.where(causal[None], score, NEG_INF)
        _, sel = lax.top_k(score, topk)
        valid = sel <= qpos[None, :, None]
        kv_sel = jax.vmap(lambda c, i: c[i])(ckv, sel)
        s = jnp.einsum('bqhc,bqkc->bqhk', ql, kv_sel).astype(jnp.float32)
        s = jnp.where(valid[:, :, None, :], s, NEG_INF)
        p = jax.nn.softmax(s, axis=-1).astype(kv_sel.dtype)
        o_lat = jnp.einsum('bqhk,bqkc->bqhc', p, kv_sel)
        o = jnp.einsum('bqhc,chd->bqhd', o_lat, w_uv)
        return o.reshape(B, Q_BLOCK, N_HEADS_A * D_V)

    starts = jnp.arange(T // Q_BLOCK) * Q_BLOCK
    out = lax.map(block, starts)
    return out.transpose(1, 0, 2, 3).reshape(B, T, N_HEADS_A * D_V)


def conv_module(u_glu, w_dw, b_dw, ln_g, ln_b, w_pw_out):
    a, g = jnp.split(u_glu, 2, axis=-1)
    u = a * jax.nn.sigmoid(g)
    y = lax.conv_general_dilated(
        u, w_dw[:, None, :].astype(u.dtype), window_strides=(1,),
        padding=((CONV_WIDTH - 1, 0),), dimension_numbers=('NWC', 'WIO', 'NWC'),
        feature_group_count=CONV_CH) + b_dw
    return jax.nn.silu(layer_norm(y, ln_g, ln_b)) @ w_pw_out


def memory_attention(q_raw, mem, mem_norm_g, w_mem_kv):
    B, T, _ = q_raw.shape
    M = mem.shape[1]
    q = q_raw.reshape(B, T, N_HEADS_M, D_HEAD_M)
    k, v = jnp.split(rms_norm(mem, mem_norm_g) @ w_mem_kv, 2, axis=-1)
    k = k.reshape(B, M, N_HEADS_M, D_HEAD_M)
    v = v.reshape(B, M, N_HEADS_M, D_HEAD_M)
    s = jnp.einsum('bthd,bmhd->bhtm', q, k).astype(jnp.float32) * (D_HEAD_M ** -0.5)
    p = jax.nn.softmax(s, axis=-1).astype(v.dtype)
    return jnp.einsum('bhtm,bmhd->bthd', p, v).reshape(B, T, N_HEADS_M * D_HEAD_M)


def hybrid_mixer(x, mem, mix_pre_g, mix_post_g, w_in,
                 q_norm_g, kv_norm_g, w_uq, w_uk, w_uv, w_idx_q, idx_ln_g, idx_ln_b, w_dsa_o,
                 w_dw, b_dw, conv_ln_g, conv_ln_b, w_conv_out,
                 mem_norm_g, w_mem_kv, w_mem_o, w_out):
    h = rms_norm(x, mix_pre_g)
    cuts = [int(c) for c in np.cumsum(SPLITS)[:-1]]
    c_q, c_kv, k_idx_raw, w_idx_raw, u_glu, q_mem, gate_logits = jnp.split(h @ w_in, cuts, axis=-1)
    y_a = dsa_attention(h, c_q, c_kv, k_idx_raw, w_idx_raw, q_norm_g, kv_norm_g, w_uq, w_uk, w_uv,
                        w_idx_q, idx_ln_g, idx_ln_b) @ w_dsa_o
    y_b = conv_module(u_glu, w_dw, b_dw, conv_ln_g, conv_ln_b, w_conv_out)
    y_m = memory_attention(q_mem, mem, mem_norm_g, w_mem_kv) @ w_mem_o
    g_a, g_b, g_m = jnp.split(jax.nn.sigmoid(gate_logits), N_BRANCH, axis=-1)
    merged = g_a * y_a + g_b * y_b + g_m * y_m
    return x + rms_norm(merged @ w_out, mix_post_g)


def setup_inputs(seed: int = 0) -> dict:
    key = jax.random.key(seed)
    ks = iter(jax.random.split(key, 40))

    def dense(shape, fan_in):
        return jax.random.normal(next(ks), (DEPTH,) + shape, jnp.float32) * (fan_in ** -0.5)

    def gain(n):
        return 1.0 + 0.02 * jax.random.normal(next(ks), (DEPTH, n), jnp.float32)

    def bias(n):
        return 0.02 * jax.random.normal(next(ks), (DEPTH, n), jnp.float32)

    x = jax.random.normal(next(ks), (BATCH, SEQ, D_MODEL), jnp.float32)
    mem = jax.random.normal(next(ks), (BATCH, N_MEM, D_MODEL), jnp.float32)
    return {
        "x": x, "mem": mem,
        "ffn1_pre_g": gain(D_MODEL), "ffn1_post_g": gain(D_MODEL),
        "ffn1_w_gu": dense((D_MODEL, 2 * D_FF), D_MODEL), "ffn1_w_down": dense((D_FF, D_MODEL), D_FF),
        "mix_pre_g": gain(D_MODEL), "mix_post_g": gain(D_MODEL),
        "w_in": dense((D_MODEL, D_IN), D_MODEL),
        "q_norm_g": gain(Q_LORA), "kv_norm_g": gain(KV_LORA),
        "w_uq": dense((Q_LORA, N_HEADS_A * D_QK), Q_LORA),
        "w_uk": dense((KV_LORA, N_HEADS_A, D_QK), KV_LORA),
        "w_uv": dense((KV_LORA, N_HEADS_A, D_V), KV_LORA),
        "w_idx_q": dense((Q_LORA, N_IDX * D_IDX), Q_LORA),
        "idx_ln_g": gain(D_IDX), "idx_ln_b": bias(D_IDX),
        "w_dsa_o": dense((N_HEADS_A * D_V, D_MODEL), N_HEADS_A * D_V),
        "w_dw": dense((CONV_WIDTH, CONV_CH), CONV_WIDTH), "b_dw": bias(CONV_CH),
        "conv_ln_g": gain(CONV_CH), "conv_ln_b": bias(CONV_CH),
        "w_conv_out": dense((CONV_CH, D_MODEL), CONV_CH),
        "mem_norm_g": gain(D_MODEL),
        "w_mem_kv": dense((D_MODEL, 2 * N_HEADS_M * D_HEAD_M), D_MODEL),
        "w_mem_o": dense((N_HEADS_M * D_HEAD_M, D_MODEL), N_HEADS_M * D_HEAD_M),
        "w_out": dense((D_MODEL, D_MODEL), D_MODEL),
        "ffn2_pre_g": gain(D_MODEL), "ffn2_post_g": gain(D_MODEL),
        "ffn2_w_gu": dense((D_MODEL, 2 * D_FF), D_MODEL), "ffn2_w_down": dense((D_FF, D_MODEL), D_FF),
    }


def reference(x, mem, ffn1_pre_g, ffn1_post_g, ffn1_w_gu, ffn1_w_down,
              mix_pre_g, mix_post_g, w_in,
              q_norm_g, kv_norm_g, w_uq, w_uk, w_uv, w_idx_q, idx_ln_g, idx_ln_b, w_dsa_o,
              w_dw, b_dw, conv_ln_g, conv_ln_b, w_conv_out,
              mem_norm_g, w_mem_kv, w_mem_o, w_out,
              ffn2_pre_g, ffn2_post_g, ffn2_w_gu, ffn2_w_down):
    for l in range(DEPTH):
        x = half_step_ffn(x, ffn1_pre_g[l], ffn1_post_g[l], ffn1_w_gu[l], ffn1_w_down[l])
        x = hybrid_mixer(x, mem, mix_pre_g[l], mix_post_g[l], w_in[l],
                         q_norm_g[l], kv_norm_g[l], w_uq[l], w_uk[l], w_uv[l], w_idx_q[l],
                         idx_ln_g[l], idx_ln_b[l], w_dsa_o[l],
                         w_dw[l], b_dw[l], conv_ln_g[l], conv_ln_b[l], w_conv_out[l],
                         mem_norm_g[l], w_mem_kv[l], w_mem_o[l], w_out[l])
        x = half_step_ffn(x, ffn2_pre_g[l], ffn2_post_g[l], ffn2_w_gu[l], ffn2_w_down[l])
    return x
```

```python
import numpy as np
from contextlib import ExitStack
import concourse.bass as bass
import concourse.mybir as mybir
from concourse.bass_utils import run_bass_kernel_spmd

F32 = mybir.dt.float32
BF16 = mybir.dt.bfloat16
AF = mybir.ActivationFunctionType
ALU = mybir.AluOpType
AX = mybir.AxisListType

D = 1024
DFF = 2816
NJ = DFF // 128
NMEM = 256
EPS = 1e-6
NBIS = 16
SLOT = 2816
import os as _os0
_SKIP = _os0.environ.get("KDBG_SKIP", "")
NBUF = 4
TBK = 512


class Op:
    __slots__ = ("eng", "fn", "deps", "sig", "cnt", "dsem", "idx", "skey", "wcnt")


class Sched:
    ENGS = ("pe", "act", "dve", "pool", "sp")

    def __init__(self):
        self.ops = []
        self.lastw = {}
        self.readers = {}
        self.ndsem = 0

    def add(self, eng, fn, reads=(), writes=(), dsem=None):
        op = Op()
        op.eng, op.fn, op.idx, op.sig, op.cnt, op.dsem = eng, fn, len(self.ops), False, 0, dsem
        deps = set()
        for k in reads:
            w = self.lastw.get(k)
            if w is not None:
                deps.add(w)
        for k in writes:
            w = self.lastw.get(k)
            if w is not None:
                deps.add(w)
            r = self.readers.get(k)
            if r:
                deps.update(r.values())
        for k in reads:
            self.readers.setdefault(k, {})[(eng, dsem if dsem is not None else -1, op.idx if dsem is not None else 0)] = op.idx
        for k in writes:
            self.lastw[k] = op.idx
            self.readers[k] = {}
        op.deps = deps
        self.ops.append(op)
        return op

    def finalize(self):
        ops = self.ops
        for op in ops:
            keep = set()
            for d in op.deps:
                p = ops[d]
                if p.dsem is None and p.eng == "pe" and op.eng == "pe" and op.dsem is None:
                    continue
                if p.dsem is not None and p.dsem == op.dsem:
                    continue
                keep.add(d)
                p.sig = True
            op.deps = keep
        cnt = {}
        for op in ops:
            if op.dsem is not None:
                op.sig = True
            if not op.sig:
                continue
            key = ("d", op.dsem) if op.dsem is not None else ("e", op.eng)
            cnt[key] = cnt.get(key, 0) + (16 if op.dsem is not None else 1)
            op.cnt = cnt[key]
            op.wcnt = op.cnt
            op.skey = key
        i = len(ops) - 1
        while i >= 0:
            op = ops[i]
            if op.dsem is not None:
                j = i
                while j - 1 >= 0 and ops[j - 1].dsem == op.dsem:
                    j -= 1
                for t in range(j, i + 1):
                    ops[t].wcnt = op.cnt
                i = j - 1
            else:
                i -= 1

    def emit(self, eng_name, eng, sems):
        waited = {}
        ops = self.ops
        for op in ops:
            if op.eng != eng_name:
                continue
            need = {}
            for d in op.deps:
                p = ops[d]
                k = p.skey
                if p.wcnt > need.get(k, 0):
                    need[k] = p.wcnt
            for k, v in need.items():
                if v > waited.get(k, 0):
                    eng.wait_ge(sems[k], v)
                    waited[k] = v
            ins = op.fn(eng)
            if op.sig:
                ins.then_inc(sems[op.skey], 16 if op.dsem is not None else 1)


def cst_layout():
    names = [("f1pre", 8), ("f1post", 8), ("mpre", 8), ("mpost", 8), ("f2pre", 8), ("f2post", 8), ("memg", 8),
             ("qng", 2), ("kvng", 2), ("ilng", 1), ("ilnb", 1), ("bdw", 4), ("clng", 4), ("clnb", 4), ("wdw", 124)]
    off = {}
    c = 0
    for n, w in names:
        off[n] = c
        c += w
    return off, c


def weight_specs():
    return {
        "w1gu": (NJ, 2048), "w1dn": (8, DFF), "w2gu": (NJ, 2048), "w2dn": (8, DFF),
        "wcq": (1, 2048), "wckv": (1, 2048), "wkidx": (1, 8 * 128 + 8 * 4),
        "wuq": (1, 1024), "widxq": (1, 1024), "wuk": (1, 2048), "wuv": (1, 2048),
        "wga": (8, 1536), "wgb": (8, 1536), "wgm": (8, 1536),
        "wu": (4, 2048), "wqm": (2, 2048), "wmk": (2, 2048), "wmv": (2, 2048), "wout": (4, 2048),
    }


def build_nc(T, NSEQ):
    NQB = T // TBK
    NKC = T // 128
    TOPK = min(256, T // 4)
    GQ0 = TOPK // 128
    assert TOPK % 128 == 0
    CO, NC_ = cst_layout()

    nc = bass.Bass("TRN2", target_bir_lowering=False)
    S = Sched()
    es = ExitStack()

    def dram(name, shape, kind="ExternalInput"):
        return nc.dram_tensor(name, list(shape), F32, kind=kind).ap()

    xT = dram("xT", [NSEQ, D, T])
    memT = dram("memT", [NSEQ, D, NMEM])
    cstd = dram("cst", [128, NC_])
    identd = dram("ident", [128, 128])
    causald = dram("causal", [128, 128])
    wd = {n: dram(n, [g, 128, f]) for n, (g, f) in weight_specs().items()}
    outT = dram("outT", [NSEQ, D, T], kind="ExternalOutput")

    def sb(name, shape, dt):
        return es.enter_context(nc.sbuf_tensor(name, list(shape), dt))

    xres = sb("xres", [128, 8, TBK], F32)
    hn = sb("hn", [128, 8, TBK], BF16)
    hT = sb("hT", [128, NJ, TBK], BF16)
    yb = sb("yb", [128, 8, TBK], F32)
    sq = sb("sq", [128, 2, TBK], BF16)
    rstd = sb("rstd", [128, TBK], F32)
    lnt = sb("lnt", [128, TBK], F32)
    sg = sb("sg", [128, 2, TBK], F32)
    tmpf = sb("tmpf", [128, 2, TBK], F32)
    rinv = sb("rinv", [128, 2, TBK], F32)
    ckvT = sb("ckvT", [128, 2, T], BF16)
    ckvtok = sb("ckvtok", [128, NKC, 256], BF16)
    kidxT = sb("kidxT", [128, T], BF16)
    cqT = sb("cqT", [128, 2, TBK], BF16)
    X4 = [sb("x4%d" % i, [128, 4, TBK], BF16) for i in range(3)]
    qidxT = sb("qidxT", [128, 4, TBK], BF16)
    widx = sb("widx", [128, 4, 4], F32)
    wdiag = sb("wdiag", [128, 2, 4, 128], BF16)
    rl = sb("rl", [128, 2, TBK], BF16)
    SC = sb("SC", [128, max(T, 2048)], F32)
    maskA = sb("maskA", [128, T], BF16)
    lohi = sb("lohi", [128, 2], F32)
    bj2 = sb("bj2", [128, 2], F32)
    bmid = sb("bmid", [128, 1], F32)
    bcnt = sb("bcnt", [128, 1], F32)
    bpred = sb("bpred", [128, 2], F32)
    bd = sb("bd", [128, 2], F32)
    sgn2 = sb("sgn2", [128, 2], F32)
    qlat = sb("qlat", [128, 2, 2, TBK], BF16)
    PT = sb("PT", [128, 3, TBK], BF16)
    olat = sb("olat", [128, 2, 2, TBK], BF16)
    uT = sb("uT", [128, 4, 32 + TBK], BF16)
    cdiag = sb("cdiag", [128, 31, 128], BF16)
    memkT = sb("memkT", [128, 4, NMEM], BF16)
    memv = sb("memv", [128, 2, 512], BF16)
    wukr = sb("wukr", [128, 8, 2, 128], BF16)
    wuvr = sb("wuvr", [128, 8, 2, 128], BF16)
    cst = sb("cstsb", [128, NC_], F32)
    ghalf = sb("ghalf", [128, 16], F32)
    identb = sb("identb", [128, 128], BF16)
    onesb = sb("onesb", [128, 128], BF16)
    causal = sb("causalsb", [128, 128], F32)
    epsc = sb("epsc", [128, 1], F32)
    ring = [sb("ring%d" % i, [128, SLOT], BF16) for i in range(NBUF)]
    psb = [es.enter_context(nc.psum_tensor("ps%d" % i, [128, 512], F32)) for i in range(7)]
    psT = es.enter_context(nc.psum_tensor("psT", [128, 1024], BF16))

    sems = {}

    def new_dsem():
        S.ndsem += 1
        return S.ndsem - 1

    def mm(bank, out, lhsT, rhs, start, stop, reads):
        S.add("pe", lambda e: e.matmul(out, lhsT, rhs, start=start, stop=stop), reads=reads, writes=[("ps", bank)])

    def tr(out, in_, reads):
        S.add("pe", lambda e: e.transpose(out, in_, identb[:]), reads=reads + [("identb",)], writes=[("ps", 7)])

    def act(out, in_, func, reads, writes, scale=1.0, bias=None, eng="act"):
        if bias is None:
            S.add(eng, lambda e: e.activation(out, in_, func, scale=scale), reads=reads, writes=writes)
        else:
            S.add(eng, lambda e: e.activation(out, in_, func, bias=bias, scale=scale), reads=reads, writes=writes)

    def tsc(out, in0, s1, op0, reads, writes, s2=None, op1=None, accum=None, eng="dve"):
        def f(e):
            if op1 is None:
                return e.tensor_scalar(out, in0, s1, None, op0)
            if accum is None:
                return e.tensor_scalar(out, in0, s1, s2, op0, op1)
            return e.tensor_scalar(out, in0, s1, s2, op0, op1, accum_out=accum)
        S.add(eng, f, reads=reads, writes=writes)

    def tt(out, in0, in1, op, reads, writes, eng="dve"):
        S.add(eng, lambda e: e.tensor_tensor(out, in0, in1, op), reads=reads, writes=writes)

    def stt(out, in0, scalar, in1, op0, op1, reads, writes):
        S.add("dve", lambda e: e.scalar_tensor_tensor(out, in0, scalar, in1, op0, op1), reads=reads, writes=writes)

    def cp(out, in_, reads, writes, eng="dve"):
        if eng == "act":
            S.add("act", lambda e: e.copy(out, in_), reads=reads, writes=writes)
        else:
            S.add(eng, lambda e: e.tensor_copy(out, in_), reads=reads, writes=writes)

    def recip(out, in_, reads, writes):
        S.add("dve", lambda e: e.reciprocal(out, in_), reads=reads, writes=writes)

    def memset(ap, v, writes, eng="pool"):
        S.add(eng, lambda e: e.memset(ap, v), writes=writes)

    def dma_plain(out, in_, reads, writes, dsem):
        S.add("sp", lambda e: e.dma_start(out, in_), reads=reads, writes=writes, dsem=dsem)

    def dma_cast(out, in_, reads, writes, dsem):
        S.add("pool", lambda e: e.dma_start(out, in_, max_dma_last_dim=8192), reads=reads, writes=writes, dsem=dsem)

    C = lambda name, i=0, n=1: cst[:, CO[name] + i: CO[name] + i + n]

    items = []

    def item(fn, w=None, g=0):
        items.append((w, g, fn))

    ring_sem = [new_dsem() for _ in range(NBUF)]

    sem_c = new_dsem()
    dma_plain(cst[:], cstd[:], [], [("cst",)], sem_c)
    dma_plain(causal[:], causald[:], [], [("causal",)], sem_c)
    sem_c2 = new_dsem()
    dma_cast(identb[:], identd[:], [], [("identb",)], sem_c2)
    dma_cast(wukr[:].rearrange("p a b c -> p (a b c)"), wd["wuk"][0], [], [("wukr",)], sem_c2)
    dma_cast(wuvr[:].rearrange("p a b c -> p (a b c)"), wd["wuv"][0], [], [("wuvr",)], sem_c2)
    memset(onesb[:], 1.0, [("onesb",)], eng="dve")
    memset(epsc[:], EPS, [("epsc",)], eng="dve")
    memset(sgn2[:, 0:1], 1.0, [("sgn2",)], eng="dve")
    memset(sgn2[:, 1:2], -1.0, [("sgn2",)], eng="dve")
    memset(uT[:], 0.0, [("uT", c) for c in range(4)], eng="dve")
    memset(kidxT[:], 0.0, [("kidxT", q) for q in range(NQB)], eng="dve")
    tsc(ghalf[:, 0:8], C("f1post", 0, 8), 0.5, ALU.mult, [("cst",)], [("ghalf",)])
    tsc(ghalf[:, 8:16], C("f2post", 0, 8), 0.5, ALU.mult, [("cst",)], [("ghalf",)])

    sqi = [0]

    def rms_stats(src_aps, src_keys, ncols, inv_n):
        n = len(src_aps)
        for i, (ap, k) in enumerate(zip(src_aps, src_keys)):
            b = sqi[0] % 2
            sqi[0] += 1
            act(sq[:, b, :ncols], ap, AF.Square, [k], [("sq", b)])
            mm(4, psb[4][:, :ncols], onesb[:], sq[:, b, :ncols], i == 0, i == n - 1, [("sq", b), ("onesb",)])
        finish_rstd(psb[4][:, :ncols], ("ps", 4), ncols, inv_n)

    def finish_rstd(ps_ap, ps_key, ncols, inv_n):
        act(lnt[:, :ncols], ps_ap, AF.Ln, [ps_key, ("epsc",)], [("lnt",)], scale=inv_n, bias=epsc[:])
        act(rstd[:, :ncols], lnt[:, :ncols], AF.Exp, [("lnt",)], [("rstd",)], scale=-0.5)

    def prenorm(gname):
        rms_stats([xres[:, kc, :] for kc in range(8)], [("xres", kc) for kc in range(8)], TBK, 1.0 / D)
        for kc in range(8):
            stt(hn[:, kc, :], xres[:, kc, :], C(gname, kc), rstd[:], ALU.mult, ALU.mult,
                [("xres", kc), ("cst",), ("rstd",)], [("hn", kc)])

    def postnorm_residual(gap_fn, gkey):
        finish_rstd(psb[4][:], ("ps", 4), TBK, 1.0 / D)
        for mc in range(8):
            b = mc % 2
            stt(tmpf[:, b, :], yb[:, mc, :], gap_fn(mc), rstd[:], ALU.mult, ALU.mult,
                [("yb", mc), gkey, ("rstd",)], [("tmpf", b)])
            tt(xres[:, mc, :], xres[:, mc, :], tmpf[:, b, :], ALU.add, [("xres", mc), ("tmpf", b)], [("xres", mc)],
               eng="pool")

    gi = [0]

    def ffn(wgu, wdn, gpre, ghoff):
        item(lambda s, k: prenorm(gpre))
        for j in range(NJ):
            def f(s, k, j=j):
                sv = s[:, 0:2048].rearrange("p (s c m) -> p s c m", s=2, c=8)
                b = gi[0] % 2
                gi[0] += 1
                for kc in range(8):
                    mm(b, psb[b][:], sv[:, 0, kc, :], hn[:, kc, :], kc == 0, kc == 7, [k, ("hn", kc)])
                for kc in range(8):
                    mm(2 + b, psb[2 + b][:], sv[:, 1, kc, :], hn[:, kc, :], kc == 0, kc == 7, [k, ("hn", kc)])
                act(sg[:, b, :], psb[b][:], AF.Silu, [("ps", b)], [("sg", b)])
                tt(hT[:, j, :], psb[2 + b][:], sg[:, b, :], ALU.mult, [("ps", 2 + b), ("sg", b)], [("hT", j)])
            item(f, wgu, j)
        for mc in range(8):
            def f(s, k, mc=mc):
                sv = s[:, 0:DFF].rearrange("p (c m) -> p c m", c=NJ)
                b = 5 + mc % 2
                for kc in range(NJ):
                    mm(b, psb[b][:], sv[:, kc, :], hT[:, kc, :], kc == 0, kc == NJ - 1, [k, ("hT", kc)])
                cp(yb[:, mc, :], psb[b][:], [("ps", b)], [("yb", mc)], eng="act")
                q = sqi[0] % 2
                sqi[0] += 1
                act(sq[:, q, :], psb[b][:], AF.Square, [("ps", b)], [("sq", q)])
                mm(4, psb[4][:], onesb[:], sq[:, q, :], mc == 0, mc == 7, [("sq", q), ("onesb",)])
            item(f, wdn, mc)
        item(lambda s, k: postnorm_residual(lambda mc: ghalf[:, ghoff + mc: ghoff + mc + 1], ("ghalf",)))

    def gated_out(wname, rhs_t, rhs_name, first):
        for mc in range(8):
            def f(s, k, mc=mc):
                gv = s[:, 0:1024].rearrange("p (c m) -> p c m", c=8)
                ov = s[:, 1024:1536].rearrange("p (c m) -> p c m", c=4)
                b = gi[0] % 2
                gi[0] += 1
                for kc in range(8):
                    mm(b, psb[b][:], gv[:, kc, :], hn[:, kc, :], kc == 0, kc == 7, [k, ("hn", kc)])
                for kc in range(4):
                    mm(2 + b, psb[2 + b][:], ov[:, kc, :], rhs_t[:, kc, :], kc == 0, kc == 3, [k, (rhs_name, kc)])
                act(sg[:, b, :], psb[b][:], AF.Sigmoid, [("ps", b)], [("sg", b)])
                if first:
                    tt(yb[:, mc, :], psb[2 + b][:], sg[:, b, :], ALU.mult, [("ps", 2 + b), ("sg", b)], [("yb", mc)])
                else:
                    tt(tmpf[:, b, :], psb[2 + b][:], sg[:, b, :], ALU.mult, [("ps", 2 + b), ("sg", b)], [("tmpf", b)])
                    tt(yb[:, mc, :], yb[:, mc, :], tmpf[:, b, :], ALU.add, [("yb", mc), ("tmpf", b)], [("yb", mc)],
                       eng="pool")
            item(f, wname, mc)

    def proj2_norm(wname, gname, dst_fn, dst_keys):
        def f(s, k):
            sv = s[:, 0:2048].rearrange("p (s c m) -> p s c m", s=2, c=8)
            for m in range(2):
                for kc in range(8):
                    mm(m, psb[m][:], sv[:, m, kc, :], hn[:, kc, :], kc == 0, kc == 7, [k, ("hn", kc)])
                cp(tmpf[:, m, :], psb[m][:], [("ps", m)], [("tmpf", m)], eng="act")
            rms_stats([tmpf[:, m, :] for m in range(2)], [("tmpf", m) for m in range(2)], TBK, 1.0 / 256)
            for m in range(2):
                stt(dst_fn(m), tmpf[:, m, :], C(gname, m), rstd[:], ALU.mult, ALU.mult,
                    [("tmpf", m), ("cst",), ("rstd",)], [dst_keys[m]])
        item(f, wname, 0)

    def mixer(seq, qb):
        t0 = qb * TBK
        nkc = 4 * (qb + 1)
        qT, oT, zT = X4[0], X4[1], X4[2]
        qmT, omT = X4[0], X4[1]
        maskT = hT

        item(lambda s, k: prenorm("mpre"))
        proj2_norm("wcq", "qng", lambda m: cqT[:, m, :], [("cqT", 0), ("cqT", 1)])
        proj2_norm("wckv", "kvng", lambda m: ckvT[:, m, t0:t0 + TBK], [("ckvT", m, qb) for m in range(2)])

        def f_tok(s, k):
            for tl in range(4):
                for m in range(2):
                    tr(psT[:, (tl % 2) * 256 + m * 128:(tl % 2) * 256 + (m + 1) * 128],
                       ckvT[:, m, t0 + tl * 128: t0 + (tl + 1) * 128], [("ckvT", m, qb)])
                cp(ckvtok[:, 4 * qb + tl, :], psT[:, (tl % 2) * 256:(tl % 2) * 256 + 256], [("ps", 7)],
                   [("ckvtok", 4 * qb + tl)])
        item(f_tok)

        def f_kidx(s, k):
            kv = s[:, 0:1024].rearrange("p (c m) -> p c m", c=8)
            wv = s[:, 1024:1056].rearrange("p (c m) -> p c m", c=8)
            if "m" in _SKIP:
                return
            for kc in range(8):
                mm(0, psb[0][:], kv[:, kc, :], hn[:, kc, :], kc == 0, kc == 7, [k, ("hn", kc)])
            if "c" in _SKIP:
                return
            cp(tmpf[:, 0, :], psb[0][:], [("ps", 0)], [("tmpf", 0)], eng="act")
            cp(sq[:, 0, :], tmpf[:, 0, :], [("tmpf", 0)], [("sq", 0)], eng="dve")
            act(sq[:, 1, :], tmpf[:, 0, :], AF.Square, [("tmpf", 0)], [("sq", 1)])
            mm(1, psb[1][:], onesb[:], sq[:, 0, :], True, True, [("sq", 0), ("onesb",)])
            mm(2, psb[2][:], onesb[:], sq[:, 1, :], True, True, [("sq", 1), ("onesb",)])
            if "l" not in _SKIP:
                ln_apply64(psb[1][0:64, :], ("ps", 1), psb[2][0:64, :], ("ps", 2))
            if "w" in _SKIP:
                return
            for qt in range(4):
                for kc in range(8):
                    mm(3, psb[3][:, qt * 4:(qt + 1) * 4], hn[:, kc, qt * 128:(qt + 1) * 128], wv[:, kc, :],
                       kc == 0, kc == 7, [k, ("hn", kc)])
            tsc(widx[:].rearrange("p a b -> p (a b)"), psb[3][:, 0:16], 0.5, ALU.mult, [("ps", 3)], [("widx",)])
        item(f_kidx, "wkidx", 0)

        def ln_apply64(psm, kmean, psv, kvar):
            P_ = slice(0, 64)
            act(tmpf[P_, 1, :], psm, AF.Copy, [kmean], [("tmpf", 1)], scale=1.0 / 64)
            tt(lnt[P_, :], tmpf[P_, 1, :], tmpf[P_, 1, :], ALU.mult, [("tmpf", 1)], [("lnt",)])
            stt(lnt[P_, :], psv, 1.0 / 64, lnt[P_, :], ALU.mult, ALU.subtract, [kvar, ("lnt",)], [("lnt",)])
            act(lnt[P_, :], lnt[P_, :], AF.Ln, [("lnt",), ("epsc",)], [("lnt",)], bias=epsc[P_, :])
            act(rstd[P_, :], lnt[P_, :], AF.Exp, [("lnt",)], [("rstd",)], scale=-0.5)
            tt(tmpf[P_, 0, :], tmpf[P_, 0, :], tmpf[P_, 1, :], ALU.subtract, [("tmpf", 0), ("tmpf", 1)], [("tmpf", 0)])
            stt(tmpf[P_, 0, :], tmpf[P_, 0, :], cst[P_, CO["ilng"]:CO["ilng"] + 1], rstd[P_, :], ALU.mult, ALU.mult,
                [("tmpf", 0), ("cst",), ("rstd",)], [("tmpf", 0)])
            tsc(kidxT[P_, t0:t0 + TBK], tmpf[P_, 0, :], cst[P_, CO["ilnb"]:CO["ilnb"] + 1], ALU.add,
                [("tmpf", 0), ("cst",)], [("kidxT", qb)])

        def f_q(s, k):
            sv = s[:, 0:1024].rearrange("p (m c n) -> p m c n", m=4, c=2)
            for m in range(4):
                b = m % 2
                for kc in range(2):
                    mm(b, psb[b][:], sv[:, m, kc, :], cqT[:, kc, :], kc == 0, kc == 1, [k, ("cqT", kc)])
                cp(qT[:, m, :], psb[b][:], [("ps", b)], [("x40", m)], eng="act" if m % 2 else "dve")
        item(f_q, "wuq", 0)

        def f_qi(s, k):
            sv = s[:, 0:1024].rearrange("p (h c n) -> p h c n", h=4, c=2)
            for h in range(4):
                b = 2 + h % 2
                for kc in range(2):
                    mm(b, psb[b][:], sv[:, h, kc, :], cqT[:, kc, :], kc == 0, kc == 1, [k, ("cqT", kc)])
                act(qidxT[:, h, :], psb[b][:], AF.Copy, [("ps", b)], [("qidxT", h)], scale=0.125)
        item(f_qi, "widxq", 0)

        def f_index(s, k):
            for qt in range(4):
                gq = 4 * qb + qt
                ncols = (gq + 1) * 128
                wb = gq % 2
                for h in range(4):
                    tsc(wdiag[:, wb, h, :], identb[:], widx[:, qt, h:h + 1], ALU.mult, [("identb",), ("widx",)],
                        [("wdiag", wb)], eng="pool", s2=0.0, op1=ALU.add)
                for kb in range(qb + 1):
                    nk = TBK if kb < qb else (qt + 1) * 128
                    c0 = kb * TBK
                    pb = 5 + (kb % 2)
                    for h in range(4):
                        mm(h, psb[h][:, :nk], qidxT[:, h, qt * 128:(qt + 1) * 128], kidxT[:, c0:c0 + nk], True, True,
                           [("qidxT", h), ("kidxT", kb)])
                        act(rl[:, h % 2, :nk], psb[h][:, :nk], AF.Relu, [("ps", h)], [("rl", h % 2)])
                        mm(pb, psb[pb][:, :nk], wdiag[:, wb, h, :], rl[:, h % 2, :nk], h == 0, h == 3,
                           [("wdiag", wb), ("rl", h % 2)])
                    if kb < qb:
                        cp(SC[:, c0:c0 + nk], psb[pb][:, :nk], [("ps", pb)], [("SC", kb)], eng="dve")
                    else:
                        if nk > 128:
                            cp(SC[:, c0:c0 + nk - 128], psb[pb][:, :nk - 128], [("ps", pb)], [("SC", kb)], eng="dve")
                        tt(SC[:, c0 + nk - 128:c0 + nk], psb[pb][:, nk - 128:nk], causal[:], ALU.add,
                           [("ps", pb), ("causal",)], [("SC", kb)])
                sck = [("SC", kb) for kb in range(qb + 1)]
                if gq >= GQ0:
                    S.add("dve", lambda e, n=gq * 128: e.tensor_reduce(lohi[:, 0:1], SC[:, 0:n], AX.X, ALU.min),
                          reads=sck, writes=[("lohi",)])
                    S.add("dve", lambda e, n=ncols: e.tensor_reduce(lohi[:, 1:2], SC[:, 0:n], AX.X, ALU.max),
                          reads=sck, writes=[("lohi",)])
                    for it in range(NBIS):
                        tsc(bj2[:], lohi[:], 0.5, ALU.mult, [("lohi",)], [("bj2",), ("bmid",)], s2=0.0, op1=ALU.add,
                            accum=bmid[:])
                        tsc(maskA[:, :ncols], SC[:, :ncols], bmid[:, 0:1], ALU.is_ge, sck + [("bmid",)],
                            [("maskA",), ("bcnt",)], s2=-(TOPK - 0.5), op1=ALU.add, accum=bcnt[:])
                        tsc(bpred[:], sgn2[:], bcnt[:, 0:1], ALU.mult, [("sgn2",), ("bcnt",)], [("bpred",)], s2=0.0,
                            op1=ALU.is_gt)
                        tsc(bd[:], lohi[:], bmid[:, 0:1], ALU.subtract, [("lohi",), ("bmid",)], [("bd",)], s2=-1.0,
                            op1=ALU.mult)
                        tt(bd[:], bd[:], bpred[:], ALU.mult, [("bd",), ("bpred",)], [("bd",)])
                        tt(lohi[:], lohi[:], bd[:], ALU.add, [("lohi",), ("bd",)], [("lohi",)])
                    tsc(maskA[:, :ncols], SC[:, :ncols], lohi[:, 0:1], ALU.is_ge, sck + [("lohi",)], [("maskA",)])
                else:
                    tsc(maskA[:, :ncols], SC[:, :ncols], -1e29, ALU.is_ge, sck, [("maskA",)])
                for k0 in range(0, gq + 1, 4):
                    n4 = min(4, gq + 1 - k0)
                    half = ((k0 // 4) % 2) * 512
                    for i in range(n4):
                        tr(psT[:, half + i * 128: half + (i + 1) * 128], maskA[:, (k0 + i) * 128:(k0 + i + 1) * 128],
                           [("maskA",)])
                    cp(maskT[:, k0:k0 + n4, qt * 128:(qt + 1) * 128],
                       psT[:, half:half + n4 * 128].rearrange("p (a b) -> p a b", a=n4), [("ps", 7)],
                       [("hT", k0 + i) for i in range(n4)], eng="act" if (k0 // 4) % 2 else "dve")
                for kc in range(gq + 1, nkc):
                    memset(maskT[:, kc, qt * 128:(qt + 1) * 128], 0.0, [("hT", kc)], eng="pool")
        item(f_index)

        pti = [0]

        def f_attn(s, k):
            for h in range(8):
                hb = h % 2
                for cc in range(2):
                    mm(4, psb[4][:], wukr[:, h, cc, :], qT[:, h // 2, :], True, True, [("wukr",), ("x40", h // 2)])
                    act(qlat[:, hb, cc, :], psb[4][:], AF.Copy, [("ps", 4)], [("qlat", hb, cc)], scale=0.125)
                for kc in range(nkc):
                    sbk = kc % 2
                    for cc in range(2):
                        mm(sbk, psb[sbk][:], ckvT[:, cc, kc * 128:(kc + 1) * 128], qlat[:, hb, cc, :], cc == 0, cc == 1,
                           [("ckvT", cc, kc // 4), ("qlat", hb, cc)])
                    p = pti[0] % 3
                    pti[0] += 1
                    act(PT[:, p, :], psb[sbk][:], AF.Exp, [("ps", sbk)], [("PT", p)])
                    tt(PT[:, p, :], PT[:, p, :], maskT[:, kc, :], ALU.mult, [("PT", p), ("hT", kc)], [("PT", p)])
                    for cc in range(2):
                        mm(2 + cc, psb[2 + cc][:], ckvtok[:, kc, cc * 128:(cc + 1) * 128], PT[:, p, :], kc == 0,
                           kc == nkc - 1, [("ckvtok", kc), ("PT", p)])
                    mm(5, psb[5][:], onesb[:], PT[:, p, :], kc == 0, kc == nkc - 1, [("onesb",), ("PT", p)])
                recip(rinv[:, hb, :], psb[5][:], [("ps", 5)], [("rinv", hb)])
                for cc in range(2):
                    tt(olat[:, hb, cc, :], psb[2 + cc][:], rinv[:, hb, :], ALU.mult, [("ps", 2 + cc), ("rinv", hb)],
                       [("olat", hb, cc)])
                for cc in range(2):
                    mm(6, psb[6][:], wuvr[:, h, cc, :], olat[:, hb, cc, :], hb == 0 and cc == 0, hb == 1 and cc == 1,
                       [("wuvr",), ("olat", hb, cc)])
                if hb == 1:
                    cp(oT[:, h // 2, :], psb[6][:], [("ps", 6)], [("x41", h // 2)], eng="act")
        item(f_attn)
        gated_out("wga", oT, "x41", True)

        for c in range(4):
            def f(s, k, c=c):
                sv = s[:, 0:2048].rearrange("p (s c m) -> p s c m", s=2, c=8)
                b = gi[0] % 2
                gi[0] += 1
                for kc in range(8):
                    mm(b, psb[b][:], sv[:, 0, kc, :], hn[:, kc, :], kc == 0, kc == 7, [k, ("hn", kc)])
                for kc in range(8):
                    mm(2 + b, psb[2 + b][:], sv[:, 1, kc, :], hn[:, kc, :], kc == 0, kc == 7, [k, ("hn", kc)])
                act(sg[:, b, :], psb[2 + b][:], AF.Sigmoid, [("ps", 2 + b)], [("sg", b)])
                tt(uT[:, c, 32:32 + TBK], psb[b][:], sg[:, b, :], ALU.mult, [("ps", b), ("sg", b)], [("uT", c)])
            item(f, "wu", c)

        def f_conv(s, k):
            for c in range(4):
                for j in range(31):
                    tsc(cdiag[:, j, :], identb[:], C("wdw", j * 4 + c), ALU.mult, [("identb",), ("cst",)],
                        [("cdiag", j)], eng="pool", s2=0.0, op1=ALU.add)
                b = 5 + c % 2
                for j in range(31):
                    mm(b, psb[b][:], cdiag[:, j, :], uT[:, c, 2 + j:2 + j + TBK], j == 0, j == 30,
                       [("cdiag", j), ("uT", c)])
                cp(uT[:, c, 0:32], uT[:, c, TBK:TBK + 32], [("uT", c)], [("uT", c)], eng="pool")
                act(SC[:, c * TBK:(c + 1) * TBK], psb[b][:], AF.Identity, [("ps", b), ("cst",)], [("SC", c)],
                    bias=C("bdw", c))
                cp(sq[:, 0, :], SC[:, c * TBK:(c + 1) * TBK], [("SC", c)], [("sq", 0)], eng="dve")
                act(sq[:, 1, :], SC[:, c * TBK:(c + 1) * TBK], AF.Square, [("SC", c)], [("sq", 1)])
                mm(0, psb[0][:], onesb[:], sq[:, 0, :], c == 0, c == 3, [("sq", 0), ("onesb",)])
                mm(1, psb[1][:], onesb[:], sq[:, 1, :], c == 0, c == 3, [("sq", 1), ("onesb",)])
            act(tmpf[:, 1, :], psb[0][:], AF.Copy, [("ps", 0)], [("tmpf", 1)], scale=1.0 / 512)
            tt(lnt[:], tmpf[:, 1, :], tmpf[:, 1, :], ALU.mult, [("tmpf", 1)], [("lnt",)])
            stt(lnt[:], psb[1][:], 1.0 / 512, lnt[:], ALU.mult, ALU.subtract, [("ps", 1), ("lnt",)], [("lnt",)])
            act(lnt[:], lnt[:], AF.Ln, [("lnt",), ("epsc",)], [("lnt",)], bias=epsc[:])
            act(rstd[:], lnt[:], AF.Exp, [("lnt",)], [("rstd",)], scale=-0.5)
            for c in range(4):
                tt(tmpf[:, 0, :], SC[:, c * TBK:(c + 1) * TBK], tmpf[:, 1, :], ALU.subtract, [("SC", c), ("tmpf", 1)],
                   [("tmpf", 0)])
                stt(tmpf[:, 0, :], tmpf[:, 0, :], C("clng", c), rstd[:], ALU.mult, ALU.mult,
                    [("tmpf", 0), ("cst",), ("rstd",)], [("tmpf", 0)])
                act(zT[:, c, :], tmpf[:, 0, :], AF.Silu, [("tmpf", 0), ("cst",)], [("x42", c)], bias=C("clnb", c))
        item(f_conv)
        gated_out("wgb", zT, "x42", False)

        for g in range(2):
            def f(s, k, g=g):
                sv = s[:, 0:2048].rearrange("p (s c m) -> p s c m", s=2, c=8)
                for m in range(2):
                    b = m
                    for kc in range(8):
                        mm(b, psb[b][:], sv[:, m, kc, :], hn[:, kc, :], kc == 0, kc == 7, [k, ("hn", kc)])
                    cp(qmT[:, 2 * g + m, :], psb[b][:], [("ps", b)], [("x40", 2 * g + m)], eng="act" if m else "dve")
            item(f, "wqm", g)

        def f_mattn(s, k):
            for h in range(4):
                hb = h % 2
                for mc2 in range(2):
                    mm(mc2, psb[mc2][:], memkT[:, h, mc2 * 128:(mc2 + 1) * 128], qmT[:, h, :], True, True,
                       [("memkT", h), ("x40", h)])
                    p = pti[0] % 3
                    pti[0] += 1
                    act(PT[:, p, :], psb[mc2][:], AF.Exp, [("ps", mc2)], [("PT", p)], scale=128.0 ** -0.5)
                    mm(2 + hb, psb[2 + hb][:], memv[:, mc2, h * 128:(h + 1) * 128], PT[:, p, :], mc2 == 0, mc2 == 1,
                       [("memv", mc2), ("PT", p)])
                    mm(5 + hb, psb[5 + hb][:], onesb[:], PT[:, p, :], mc2 == 0, mc2 == 1, [("onesb",), ("PT", p)])
                recip(rinv[:, hb, :], psb[5 + hb][:], [("ps", 5 + hb)], [("rinv", hb)])
                tt(omT[:, h, :], psb[2 + hb][:], rinv[:, hb, :], ALU.mult, [("ps", 2 + hb), ("rinv", hb)], [("x41", h)])
        item(f_mattn)
        gated_out("wgm", omT, "x41", False)

        def f_mb(s, k):
            for kc in range(8):
                cp(hn[:, kc, :], yb[:, kc, :], [("yb", kc)], [("hn", kc)], eng="act" if kc % 2 else "dve")
        item(f_mb)
        for g in range(4):
            def f(s, k, g=g):
                sv = s[:, 0:2048].rearrange("p (s c m) -> p s c m", s=2, c=8)
                for m in range(2):
                    mc = 2 * g + m
                    b = 5 + m
                    for kc in range(8):
                        mm(b, psb[b][:], sv[:, m, kc, :], hn[:, kc, :], kc == 0, kc == 7, [k, ("hn", kc)])
                    cp(yb[:, mc, :], psb[b][:], [("ps", b)], [("yb", mc)], eng="act")
                    q = sqi[0] % 2
                    sqi[0] += 1
                    act(sq[:, q, :], psb[b][:], AF.Square, [("ps", b)], [("sq", q)])
                    mm(4, psb[4][:], onesb[:], sq[:, q, :], mc == 0, mc == 7, [("sq", q), ("onesb",)])
            item(f, "wout", g)
        item(lambda s, k: postnorm_residual(lambda mc: C("mpost", mc), ("cst",)))

    def mem_prep(seq):
        sem = new_dsem()

        def f_load(s, k):
            for kc in range(8):
                dma_plain(SC[:, kc * NMEM:(kc + 1) * NMEM], memT[seq, kc * 128:(kc + 1) * 128, :], [],
                          [("SC", kc // 2)], sem)
            rms_stats([SC[:, kc * NMEM:(kc + 1) * NMEM] for kc in range(8)], [("SC", kc // 2) for kc in range(8)], NMEM,
                      1.0 / D)
            mn = X4[2][:].rearrange("p a b -> p (a b)")
            for kc in range(8):
                stt(mn[:, kc * NMEM:(kc + 1) * NMEM], SC[:, kc * NMEM:(kc + 1) * NMEM], C("memg", kc), rstd[:, :NMEM],
                    ALU.mult, ALU.mult, [("SC", kc // 2), ("cst",), ("rstd",)], [("x42", kc // 2)])
        item(f_load)
        for g in range(2):
            def f(s, k, g=g):
                sv = s[:, 0:2048].rearrange("p (s c m) -> p s c m", s=2, c=8)
                mn = X4[2][:].rearrange("p a b -> p (a b)")
                for m in range(2):
                    b = m
                    for kc in range(8):
                        mm(b, psb[b][:, :NMEM], sv[:, m, kc, :], mn[:, kc * NMEM:(kc + 1) * NMEM], kc == 0, kc == 7,
                           [k, ("x42", kc // 2)])
                    cp(memkT[:, 2 * g + m, :], psb[b][:, :NMEM], [("ps", b)], [("memkT", 2 * g + m)],
                       eng="act" if m else "dve")
            item(f, "wmk", g)
        for g in range(2):
            def f(s, k, g=g):
                sv = s[:, 0:2048].rearrange("p (c n) -> p c n", c=4)
                mn = X4[2][:].rearrange("p a b -> p (a b)")
                for mc2 in range(2):
                    for kl in range(4):
                        kc = 4 * g + kl
                        mm(2 + mc2, psb[2 + mc2][:], mn[:, kc * NMEM + mc2 * 128: kc * NMEM + (mc2 + 1) * 128],
                           sv[:, kl, :], kc == 0, kc == 7, [k, ("x42", kc // 2)])
                    if g == 1:
                        cp(memv[:, mc2, :], psb[2 + mc2][:], [("ps", 2 + mc2)], [("memv", mc2)],
                           eng="act" if mc2 else "dve")
            item(f, "wmv", g)

    out_sems = []
    for seq in range(NSEQ):
        mem_prep(seq)
        if seq > 0:
            item(lambda s, k: memset(uT[:], 0.0, [("uT", c) for c in range(4)], eng="dve"))
        for qb in range(NQB):
            t0 = qb * TBK
            semx = new_dsem()

            def f_x(s, k, seq=seq, t0=t0, semx=semx):
                for kc in range(8):
                    dma_plain(xres[:, kc, :], xT[seq, kc * 128:(kc + 1) * 128, t0:t0 + TBK], [], [("xres", kc)], semx)
            item(f_x)
            ffn("w1gu", "w1dn", "f1pre", 0)
            mixer(seq, qb)
            ffn("w2gu", "w2dn", "f2pre", 8)
            semo = new_dsem()
            out_sems.append(semo)

            def f_o(s, k, seq=seq, t0=t0, semo=semo):
                for kc in range(8):
                    dma_plain(outT[seq, kc * 128:(kc + 1) * 128, t0:t0 + TBK], xres[:, kc, :], [("xres", kc)],
                              [("out", seq, t0, kc)], semo)
            item(f_o)

    loads = [(i, it) for i, it in enumerate(items) if it[0] is not None]
    slot_of = {}
    nl = [0]

    def issue_load():
        li = nl[0]
        if li >= len(loads):
            return
        i, (w, g, _) = loads[li]
        sl = li % NBUF
        n = weight_specs()[w][1]
        dma_cast(ring[sl][:, 0:n], wd[w][g], [], [("ring", sl)], ring_sem[sl])
        slot_of[i] = sl
        nl[0] += 1

    import os as _os
    _n = int(_os.environ.get("KDBG_N", "0"))
    if _n:
        items[:] = items[:_n]
        loads[:] = [(i, it) for i, it in enumerate(items) if it[0] is not None]
    for _ in range(NBUF - 1):
        issue_load()
    for i, (w, g, fn) in enumerate(items):
        if w is not None:
            issue_load()
            sl = slot_of[i]
            fn(ring[sl], ("ring", sl))
        else:
            fn(None, None)

    fin_reads = []
    for seq in range(NSEQ):
        for qb in range(NQB):
            for kc in range(8):
                fin_reads.append(("out", seq, qb * TBK, kc))
    S.add("sp", lambda e: e.nop(), reads=fin_reads, writes=[])

    S.finalize()
    with ExitStack() as es2:
        for k in set(op.skey for op in S.ops if op.sig):
            sems[k] = es2.enter_context(nc.semaphore("s_%s_%s" % (k[0], k[1])))
        block = es2.enter_context(nc.Block())

        @block.tensor
        def _(e):
            S.emit("pe", e, sems)

        @block.scalar
        def _(e):
            S.emit("act", e, sems)

        @block.vector
        def _(e):
            S.emit("dve", e, sems)

        @block.gpsimd
        def _(e):
            S.emit("pool", e, sems)

        @block.sync
        def _(e):
            S.emit("sp", e, sems)
    es.close()
    return nc


def lhsT_tiles(W, mchunk=128):
    K, M = W.shape
    return np.ascontiguousarray(W.reshape(K // 128, 128, M // mchunk, mchunk).transpose(2, 1, 0, 3))


def col_tile(v):
    n = v.shape[0]
    if n < 128:
        o = np.zeros((128, 1), np.float32)
        o[:n, 0] = v
        return o
    return np.ascontiguousarray(v.reshape(n // 128, 128).T)


def prep_weights(p):
    f = np.float32
    w = {}
    for i in (1, 2):
        gu = p["ffn%d_w_gu" % i][0]
        g = lhsT_tiles(gu[:, :DFF])
        u = lhsT_tiles(gu[:, DFF:])
        w["w%dgu" % i] = np.stack([g, u], axis=2).reshape(NJ, 128, 2048)
        w["w%ddn" % i] = lhsT_tiles(p["ffn%d_w_down" % i][0]).reshape(8, 128, DFF)
    win = p["w_in"][0]
    o_cq, o_ckv, o_ki, o_wi, o_u, o_qm, o_g = 0, 256, 512, 576, 580, 1604, 2116
    w["wcq"] = lhsT_tiles(win[:, o_cq:o_cq + 256]).transpose(1, 0, 2, 3).reshape(1, 128, 2048)
    w["wckv"] = lhsT_tiles(win[:, o_ckv:o_ckv + 256]).transpose(1, 0, 2, 3).reshape(1, 128, 2048)
    kip = np.zeros((D, 128), f)
    kip[:, :64] = win[:, o_ki:o_ki + 64]
    ki = lhsT_tiles(kip)[0].reshape(128, 1024)
    wi = lhsT_tiles(win[:, o_wi:o_wi + 4], 4)[0].reshape(128, 32)
    w["wkidx"] = np.concatenate([ki, wi], axis=1)[None]
    w["wuq"] = lhsT_tiles(p["w_uq"][0]).transpose(1, 0, 2, 3).reshape(1, 128, 1024)
    wiq = np.zeros((256, 4, 128), f)
    wiq[:, :, :64] = p["w_idx_q"][0].reshape(256, 4, 64)
    w["widxq"] = lhsT_tiles(wiq.reshape(256, 512)).transpose(1, 0, 2, 3).reshape(1, 128, 1024)
    wuk = p["w_uk"][0]
    wuv = p["w_uv"][0]
    uk = np.zeros((128, 8, 2, 128), f)
    uv = np.zeros((128, 8, 2, 128), f)
    for h in range(8):
        r0 = (h % 2) * 64
        for cc in range(2):
            uk[r0:r0 + 64, h, cc, :] = wuk[cc * 128:(cc + 1) * 128, h, :].T
            uv[:, h, cc, r0:r0 + 64] = wuv[cc * 128:(cc + 1) * 128, h, :]
    w["wuk"] = uk.reshape(1, 128, 2048)
    w["wuv"] = uv.reshape(1, 128, 2048)
    for bi, (nm, wo) in enumerate((("wga", "w_dsa_o"), ("wgb", "w_conv_out"), ("wgm", "w_mem_o"))):
        gt = lhsT_tiles(win[:, o_g + bi * D:o_g + (bi + 1) * D]).reshape(8, 128, 1024)
        ot = lhsT_tiles(p[wo][0]).reshape(8, 128, 512)
        w[nm] = np.concatenate([gt, ot], axis=2)
    ua = lhsT_tiles(win[:, o_u:o_u + 512])
    ug = lhsT_tiles(win[:, o_u + 512:o_u + 1024])
    w["wu"] = np.stack([ua, ug], axis=2).reshape(4, 128, 2048)
    qm = lhsT_tiles(win[:, o_qm:o_qm + 512])
    w["wqm"] = qm.reshape(2, 2, 128, 8, 128).transpose(0, 2, 1, 3, 4).reshape(2, 128, 2048)
    mkv = p["w_mem_kv"][0]
    mk = lhsT_tiles(mkv[:, :512])
    w["wmk"] = mk.reshape(2, 2, 128, 8, 128).transpose(0, 2, 1, 3, 4).reshape(2, 128, 2048)
    mv = mkv[:, 512:].reshape(8, 128, 512)
    w["wmv"] = mv.reshape(2, 4, 128, 512).transpose(0, 2, 1, 3).reshape(2, 128, 2048)
    wo = lhsT_tiles(p["w_out"][0])
    w["wout"] = wo.reshape(4, 2, 128, 8, 128).transpose(0, 2, 1, 3, 4).reshape(4, 128, 2048)
    CO, NC_ = cst_layout()
    cst = np.zeros((128, NC_), f)

    def put(name, v):
        t = col_tile(np.asarray(v, f))
        cst[:, CO[name]:CO[name] + t.shape[1]] = t
    put("f1pre", p["ffn1_pre_g"][0]); put("f1post", p["ffn1_post_g"][0])
    put("mpre", p["mix_pre_g"][0]); put("mpost", p["mix_post_g"][0])
    put("f2pre", p["ffn2_pre_g"][0]); put("f2post", p["ffn2_post_g"][0])
    put("memg", p["mem_norm_g"][0]); put("qng", p["q_norm_g"][0]); put("kvng", p["kv_norm_g"][0])
    put("ilng", p["idx_ln_g"][0]); put("ilnb", p["idx_ln_b"][0])
    put("bdw", p["b_dw"][0]); put("clng", p["conv_ln_g"][0]); put("clnb", p["conv_ln_b"][0])
    wdw = p["w_dw"][0]
    cst[:, CO["wdw"]:CO["wdw"] + 124] = wdw.reshape(31, 4, 128).transpose(2, 0, 1).reshape(128, 124)
    w["cst"] = cst
    w["ident"] = np.eye(128, dtype=f)
    cm = np.zeros((128, 128), f)
    cm[np.triu_indices(128, 1)] = -1e30
    w["causal"] = cm
    return {k: np.ascontiguousarray(v, dtype=f) for k, v in w.items()}


_NC_CACHE = {}


def run(inputs, T, B, n_cores, runner=None):
    p = {k: np.asarray(v) for k, v in inputs.items()}
    NSEQ = B // n_cores
    w = prep_weights(p)
    x = np.asarray(p["x"], np.float32)
    mem = np.asarray(p["mem"], np.float32)
    xT = np.ascontiguousarray(x.transpose(0, 2, 1))
    mT = np.ascontiguousarray(mem.transpose(0, 2, 1))
    key = (T, NSEQ)
    if key not in _NC_CACHE:
        _NC_CACHE[key] = build_nc(T, NSEQ)
    nc = _NC_CACHE[key]
    in_maps = []
    for c in range(n_cores):
        m = dict(w)
        m["xT"] = xT[c * NSEQ:(c + 1) * NSEQ]
        m["memT"] = mT[c * NSEQ:(c + 1) * NSEQ]
        in_maps.append(m)
    if runner is None:
        res = run_bass_kernel_spmd(nc, in_maps, core_ids=list(range(n_cores)))
        outs = [r["outT"] for r in res.results]
    else:
        outs = runner(nc, in_maps)
    oT = np.concatenate(outs, axis=0)
    return np.ascontiguousarray(oT.transpose(0, 2, 1)).astype(np.float32)


def kernel(**inputs):
    return run(inputs, 2048, 16, 8)
```

```python
import numpy as np
from contextlib import ExitStack
import concourse.bass as bass
import concourse.mybir as mybir
from concourse.bass_utils import run_bass_kernel_spmd

F32 = mybir.dt.float32
BF16 = mybir.dt.bfloat16
AF = mybir.ActivationFunctionType
ALU = mybir.AluOpType
AX = mybir.AxisListType

D = 1024
DFF = 2816
NJ = DFF // 128
NMEM = 256
EPS = 1e-6
NBIS = 14
SLOT = 2816
import os as _os0
_SKIP = _os0.environ.get("KDBG_SKIP", "")
NBUF = 4
TBK = 512


class Op:
    __slots__ = ("eng", "fn", "deps", "sig", "cnt", "dsem", "idx", "skey", "wcnt")


class Sched:
    ENGS = ("pe", "act", "dve", "pool", "sp")

    def __init__(self):
        self.ops = []
        self.lastw = {}
        self.readers = {}
        self.ndsem = 0

    def add(self, eng, fn, reads=(), writes=(), dsem=None):
        op = Op()
        op.eng, op.fn, op.idx, op.sig, op.cnt, op.dsem = eng, fn, len(self.ops), False, 0, dsem
        deps = set()
        for k in reads:
            w = self.lastw.get(k)
            if w is not None:
                deps.add(w)
        for k in writes:
            w = self.lastw.get(k)
            if w is not None:
                deps.add(w)
            r = self.readers.get(k)
            if r:
                deps.update(r.values())
        for k in reads:
            self.readers.setdefault(k, {})[(eng, dsem if dsem is not None else -1, op.idx if dsem is not None else 0)] = op.idx
        for k in writes:
            self.lastw[k] = op.idx
            self.readers[k] = {}
        op.deps = deps
        self.ops.append(op)
        return op

    def finalize(self):
        ops = self.ops
        for op in ops:
            keep = set()
            for d in op.deps:
                p = ops[d]
                if p.dsem is None and p.eng == "pe" and op.eng == "pe" and op.dsem is None:
                    continue
                if p.dsem is not None and p.dsem == op.dsem:
                    continue
                keep.add(d)
                p.sig = True
            op.deps = keep
        cnt = {}
        for op in ops:
            if op.dsem is not None:
                op.sig = True
            if not op.sig:
                continue
            key = ("d", op.dsem) if op.dsem is not None else ("e", op.eng)
            cnt[key] = cnt.get(key, 0) + (16 if op.dsem is not None else 1)
            op.cnt = cnt[key]
            op.wcnt = op.cnt
            op.skey = key
        i = len(ops) - 1
        while i >= 0:
            op = ops[i]
            if op.dsem is not None:
                j = i
                while j - 1 >= 0 and ops[j - 1].dsem == op.dsem:
                    j -= 1
                for t in range(j, i + 1):
                    ops[t].wcnt = op.cnt
                i = j - 1
            else:
                i -= 1

    def emit(self, eng_name, eng, sems):
        waited = {}
        ops = self.ops
        for op in ops:
            if op.eng != eng_name:
                continue
            need = {}
            for d in op.deps:
                p = ops[d]
                k = p.skey
                if p.wcnt > need.get(k, 0):
                    need[k] = p.wcnt
            for k, v in need.items():
                if v > waited.get(k, 0):
                    eng.wait_ge(sems[k], v)
                    waited[k] = v
            ins = op.fn(eng)
            if op.sig:
                ins.then_inc(sems[op.skey], 16 if op.dsem is not None else 1)


def cst_layout():
    names = [("f1pre", 8), ("f1post", 8), ("mpre", 8), ("mpost", 8), ("f2pre", 8), ("f2post", 8), ("memg", 8),
             ("qng", 2), ("kvng", 2), ("ilng", 1), ("ilnb", 1), ("bdw", 4), ("clng", 4), ("clnb", 4), ("wdw", 124), ("pw", NBIS)]
    off = {}
    c = 0
    for n, w in names:
        off[n] = c
        c += w
    return off, c


def weight_specs():
    return {
        "w1gu": (NJ, 2048), "w1dn": (8, DFF), "w2gu": (NJ, 2048), "w2dn": (8, DFF),
        "wcq": (1, 2048), "wckv": (1, 2048), "wkidx": (1, 8 * 128 + 8 * 4),
        "wuq": (1, 1024), "widxq": (1, 1024), "wuk": (1, 2048), "wuv": (1, 2048),
        "wga": (8, 1536), "wgb": (8, 1536), "wgm": (8, 1536),
        "wu": (4, 2048), "wqm": (2, 2048), "wmk": (2, 2048), "wmv": (2, 2048), "wout": (4, 2048),
    }


def build_nc(T, NSEQ):
    NQB = T // TBK
    NKC = T // 128
    TOPK = min(256, T // 4)
    GQ0 = TOPK // 128
    assert TOPK % 128 == 0
    CO, NC_ = cst_layout()

    nc = bass.Bass("TRN2", target_bir_lowering=False)
    S = Sched()
    es = ExitStack()

    def dram(name, shape, kind="ExternalInput"):
        return nc.dram_tensor(name, list(shape), F32, kind=kind).ap()

    xT = dram("xT", [NSEQ, D, T])
    memT = dram("memT", [NSEQ, D, NMEM])
    cstd = dram("cst", [128, NC_])
    identd = dram("ident", [128, 128])
    causald = dram("causal", [128, 128])
    wd = {n: dram(n, [g, 128, f]) for n, (g, f) in weight_specs().items()}
    outT = dram("outT", [NSEQ, D, T], kind="ExternalOutput")

    def sb(name, shape, dt):
        return es.enter_context(nc.sbuf_tensor(name, list(shape), dt))

    xres = sb("xres", [128, 8, TBK], F32)
    hn = sb("hn", [128, 8, TBK], BF16)
    hT = sb("hT", [128, NJ, TBK], BF16)
    yb = sb("yb", [128, 8, TBK], F32)
    sq = sb("sq", [128, 2, TBK], BF16)
    rstd = sb("rstd", [128, TBK], F32)
    lnt = sb("lnt", [128, TBK], F32)
    sg = sb("sg", [128, 2, TBK], F32)
    tmpf = sb("tmpf", [128, 2, TBK], F32)
    rinv = sb("rinv", [128, 2, TBK], F32)
    ckvT = sb("ckvT", [128, 2, T], BF16)
    ckvtok = sb("ckvtok", [128, NKC, 256], BF16)
    kidxT = sb("kidxT", [128, T], BF16)
    cqT = sb("cqT", [128, 2, TBK], BF16)
    X4 = [sb("x4%d" % i, [128, 4, TBK], BF16) for i in range(3)]
    qidxT = sb("qidxT", [128, 4, TBK], BF16)
    widx = sb("widx", [128, 4, 4], F32)
    wdiag = sb("wdiag", [128, 2, 4, 128], BF16)
    rl = sb("rl", [128, 4, TBK], BF16)
    SCs = [sb("SC%d" % i, [128, max(T, 2048) if i == 0 else T], F32) for i in range(2)]
    maskAs = [sb("maskA%d" % i, [128, T], BF16) for i in range(2)]
    blo = sb("blo", [128, 2, 2], F32)
    bw0 = sb("bw0", [128, 2], F32)
    BH = sb("BH", [128, 2, NBIS], F32)
    BH2 = sb("BH2", [128, 2, NBIS], F32)
    bmid = sb("bmid", [128, 2], F32)
    bcnt = sb("bcnt", [128, 2], F32)
    bu = sb("bu", [128, 2], F32)
    qlat = sb("qlat", [128, 2, 2, TBK], BF16)
    PT = sb("PT", [128, 3, TBK], BF16)
    olat = sb("olat", [128, 2, 2, TBK], BF16)
    uT = sb("uT", [128, 4, 32 + TBK], BF16)
    cdiag = sb("cdiag", [128, 31, 128], BF16)
    memkT = sb("memkT", [128, 4, NMEM], BF16)
    memv = sb("memv", [128, 2, 512], BF16)
    wukr = sb("wukr", [128, 8, 2, 128], BF16)
    wuvr = sb("wuvr", [128, 8, 2, 128], BF16)
    cst = sb("cstsb", [128, NC_], F32)
    ghalf = sb("ghalf", [128, 16], F32)
    identb = sb("identb", [128, 128], BF16)
    onesb = sb("onesb", [128, 128], BF16)
    causal = sb("causalsb", [128, 128], F32)
    epsc = sb("epsc", [128, 1], F32)
    ring = [sb("ring%d" % i, [128, SLOT], BF16) for i in range(NBUF)]
    psb = [es.enter_context(nc.psum_tensor("ps%d" % i, [128, 512], F32)) for i in range(7)]
    psT = es.enter_context(nc.psum_tensor("psT", [128, 1024], BF16))

    sems = {}

    def new_dsem():
        S.ndsem += 1
        return S.ndsem - 1

    def mm(bank, out, lhsT, rhs, start, stop, reads):
        S.add("pe", lambda e: e.matmul(out, lhsT, rhs, start=start, stop=stop), reads=reads, writes=[("ps", bank)])

    def tr(out, in_, reads):
        S.add("pe", lambda e: e.transpose(out, in_, identb[:]), reads=reads + [("identb",)], writes=[("ps", 7)])

    def act(out, in_, func, reads, writes, scale=1.0, bias=None, eng="act"):
        if bias is None:
            S.add(eng, lambda e: e.activation(out, in_, func, scale=scale), reads=reads, writes=writes)
        else:
            S.add(eng, lambda e: e.activation(out, in_, func, bias=bias, scale=scale), reads=reads, writes=writes)

    def tsc(out, in0, s1, op0, reads, writes, s2=None, op1=None, accum=None, eng="dve"):
        def f(e):
            if op1 is None:
                return e.tensor_scalar(out, in0, s1, None, op0)
            if accum is None:
                return e.tensor_scalar(out, in0, s1, s2, op0, op1)
            return e.tensor_scalar(out, in0, s1, s2, op0, op1, accum_out=accum)
        S.add(eng, f, reads=reads, writes=writes)

    def tt(out, in0, in1, op, reads, writes, eng="dve"):
        S.add(eng, lambda e: e.tensor_tensor(out, in0, in1, op), reads=reads, writes=writes)

    def stt(out, in0, scalar, in1, op0, op1, reads, writes):
        S.add("dve", lambda e: e.scalar_tensor_tensor(out, in0, scalar, in1, op0, op1), reads=reads, writes=writes)

    def cp(out, in_, reads, writes, eng="dve"):
        if eng == "act":
            S.add("act", lambda e: e.copy(out, in_), reads=reads, writes=writes)
        else:
            S.add(eng, lambda e: e.tensor_copy(out, in_), reads=reads, writes=writes)

    def recip(out, in_, reads, writes):
        S.add("dve", lambda e: e.reciprocal(out, in_), reads=reads, writes=writes)

    def memset(ap, v, writes, eng="pool"):
        S.add(eng, lambda e: e.memset(ap, v), writes=writes)

    def dma_plain(out, in_, reads, writes, dsem):
        S.add("sp", lambda e: e.dma_start(out, in_), reads=reads, writes=writes, dsem=dsem)

    def dma_cast(out, in_, reads, writes, dsem):
        S.add("pool", lambda e: e.dma_start(out, in_, max_dma_last_dim=8192), reads=reads, writes=writes, dsem=dsem)

    C = lambda name, i=0, n=1: cst[:, CO[name] + i: CO[name] + i + n]

    items = []

    def item(fn, w=None, g=0):
        items.append((w, g, fn))

    ring_sem = [new_dsem() for _ in range(NBUF)]

    sem_c = new_dsem()
    dma_plain(cst[:], cstd[:], [], [("cst",)], sem_c)
    dma_plain(causal[:], causald[:], [], [("causal",)], sem_c)
    sem_c2 = new_dsem()
    dma_cast(identb[:], identd[:], [], [("identb",)], sem_c2)
    dma_cast(wukr[:].rearrange("p a b c -> p (a b c)"), wd["wuk"][0], [], [("wukr",)], sem_c2)
    dma_cast(wuvr[:].rearrange("p a b c -> p (a b c)"), wd["wuv"][0], [], [("wuvr",)], sem_c2)
    memset(onesb[:], 1.0, [("onesb",)], eng="dve")
    memset(epsc[:], EPS, [("epsc",)], eng="dve")
    memset(uT[:], 0.0, [("uT", c) for c in range(4)], eng="dve")
    memset(kidxT[:], 0.0, [("kidxT", q) for q in range(NQB)], eng="dve")
    tsc(ghalf[:, 0:8], C("f1post", 0, 8), 0.5, ALU.mult, [("cst",)], [("ghalf",)])
    tsc(ghalf[:, 8:16], C("f2post", 0, 8), 0.5, ALU.mult, [("cst",)], [("ghalf",)])

    sqi = [0]

    def rms_stats(src_aps, src_keys, ncols, inv_n):
        n = len(src_aps)
        for i, (ap, k) in enumerate(zip(src_aps, src_keys)):
            b = sqi[0] % 2
            sqi[0] += 1
            act(sq[:, b, :ncols], ap, AF.Square, [k], [("sq", b)])
            mm(4, psb[4][:, :ncols], onesb[:], sq[:, b, :ncols], i == 0, i == n - 1, [("sq", b), ("onesb",)])
        finish_rstd(psb[4][:, :ncols], ("ps", 4), ncols, inv_n)

    def finish_rstd(ps_ap, ps_key, ncols, inv_n):
        act(lnt[:, :ncols], ps_ap, AF.Ln, [ps_key, ("epsc",)], [("lnt",)], scale=inv_n, bias=epsc[:])
        act(rstd[:, :ncols], lnt[:, :ncols], AF.Exp, [("lnt",)], [("rstd",)], scale=-0.5)

    def prenorm(gname):
        rms_stats([xres[:, kc, :] for kc in range(8)], [("xres", kc) for kc in range(8)], TBK, 1.0 / D)
        for kc in range(8):
            stt(hn[:, kc, :], xres[:, kc, :], C(gname, kc), rstd[:], ALU.mult, ALU.mult,
                [("xres", kc), ("cst",), ("rstd",)], [("hn", kc)])

    def postnorm_residual(gap_fn, gkey):
        finish_rstd(psb[4][:], ("ps", 4), TBK, 1.0 / D)
        for mc in range(8):
            b = mc % 2
            stt(tmpf[:, b, :], yb[:, mc, :], gap_fn(mc), rstd[:], ALU.mult, ALU.mult,
                [("yb", mc), gkey, ("rstd",)], [("tmpf", b)])
            tt(xres[:, mc, :], xres[:, mc, :], tmpf[:, b, :], ALU.add, [("xres", mc), ("tmpf", b)], [("xres", mc)],
               eng="pool")

    gi = [0]
    pend_stats = []

    def flush_stats():
        for (q, mc) in pend_stats:
            mm(4, psb[4][:], onesb[:], sq[:, q, :], mc == 0, mc == 7, [("sq", q), ("onesb",)])
        del pend_stats[:]

    def ffn(wgu, wdn, gpre, ghoff):
        item(lambda s, k: prenorm(gpre))
        for j in range(NJ):
            def f(s, k, j=j):
                sv = s[:, 0:2048].rearrange("p (s c m) -> p s c m", s=2, c=8)
                b = gi[0] % 2
                gi[0] += 1
                for kc in range(8):
                    mm(b, psb[b][:], sv[:, 0, kc, :], hn[:, kc, :], kc == 0, kc == 7, [k, ("hn", kc)])
                for kc in range(8):
                    mm(2 + b, psb[2 + b][:], sv[:, 1, kc, :], hn[:, kc, :], kc == 0, kc == 7, [k, ("hn", kc)])
                act(sg[:, b, :], psb[b][:], AF.Silu, [("ps", b)], [("sg", b)])
                tt(hT[:, j, :], psb[2 + b][:], sg[:, b, :], ALU.mult, [("ps", 2 + b), ("sg", b)], [("hT", j)])
            item(f, wgu, j)
        for mc in range(8):
            def f(s, k, mc=mc):
                sv = s[:, 0:DFF].rearrange("p (c m) -> p c m", c=NJ)
                b = 5 + mc % 2
                for kc in range(NJ):
                    mm(b, psb[b][:], sv[:, kc, :], hT[:, kc, :], kc == 0, kc == NJ - 1, [k, ("hT", kc)])
                flush_stats()
                q = sqi[0] % 2
                sqi[0] += 1
                act(sq[:, q, :], psb[b][:], AF.Square, [("ps", b)], [("sq", q)])
                cp(yb[:, mc, :], psb[b][:], [("ps", b)], [("yb", mc)], eng="act")
                pend_stats.append((q, mc))
            item(f, wdn, mc)

        def f_post(s, k):
            flush_stats()
            postnorm_residual(lambda mc: ghalf[:, ghoff + mc: ghoff + mc + 1], ("ghalf",))
        item(f_post)

    def gated_out(wname, rhs_t, rhs_name, first, nbg=0):
        ce = "pool" if nbg else "dve"
        for mc in range(8):
            def f(s, k, mc=mc):
                gv = s[:, 0:1024].rearrange("p (c m) -> p c m", c=8)
                ov = s[:, 1024:1536].rearrange("p (c m) -> p c m", c=4)
                b = gi[0] % 2
                gi[0] += 1
                for kc in range(8):
                    mm(b, psb[b][:], gv[:, kc, :], hn[:, kc, :], kc == 0, kc == 7, [k, ("hn", kc)])
                for kc in range(4):
                    mm(2 + b, psb[2 + b][:], ov[:, kc, :], rhs_t[:, kc, :], kc == 0, kc == 3, [k, (rhs_name, kc)])
                act(sg[:, b, :], psb[b][:], AF.Sigmoid, [("ps", b)], [("sg", b)])
                if "g" in _SKIP and first:
                    tt(yb[:, mc, :], psb[2 + b][:], sg[:, b, :], ALU.mult, [("ps", 2 + b), ("sg", b)], [("yb", mc)])
                    bgstep_ref[0](nbg)
                elif nbg and "g" not in _SKIP:
                    cp(tmpf[:, b, :], psb[2 + b][:], [("ps", 2 + b)], [("tmpf", b)], eng="act")
                    if first:
                        tt(yb[:, mc, :], tmpf[:, b, :], sg[:, b, :], ALU.mult, [("tmpf", b), ("sg", b)], [("yb", mc)],
                           eng="pool")
                    else:
                        tt(tmpf[:, b, :], tmpf[:, b, :], sg[:, b, :], ALU.mult, [("tmpf", b), ("sg", b)],
                           [("tmpf", b)], eng="pool")
                        tt(yb[:, mc, :], yb[:, mc, :], tmpf[:, b, :], ALU.add, [("yb", mc), ("tmpf", b)],
                           [("yb", mc)], eng="pool")
                    bgstep_ref[0](nbg)
                else:
                    tt(tmpf[:, b, :], psb[2 + b][:], sg[:, b, :], ALU.mult, [("ps", 2 + b), ("sg", b)], [("tmpf", b)])
                    tt(yb[:, mc, :], yb[:, mc, :], tmpf[:, b, :], ALU.add, [("yb", mc), ("tmpf", b)], [("yb", mc)],
                       eng="pool")
            item(f, wname, mc)

    bgstep_ref = [lambda n: None]

    def proj2_norm(wname, gname, dst_fn, dst_keys):
        def f(s, k):
            sv = s[:, 0:2048].rearrange("p (s c m) -> p s c m", s=2, c=8)
            for m in range(2):
                for kc in range(8):
                    mm(m, psb[m][:], sv[:, m, kc, :], hn[:, kc, :], kc == 0, kc == 7, [k, ("hn", kc)])
                cp(tmpf[:, m, :], psb[m][:], [("ps", m)], [("tmpf", m)], eng="act")
            rms_stats([tmpf[:, m, :] for m in range(2)], [("tmpf", m) for m in range(2)], TBK, 1.0 / 256)
            for m in range(2):
                stt(dst_fn(m), tmpf[:, m, :], C(gname, m), rstd[:], ALU.mult, ALU.mult,
                    [("tmpf", m), ("cst",), ("rstd",)], [dst_keys[m]])
        item(f, wname, 0)

    def mixer(seq, qb):
        t0 = qb * TBK
        nkc = 4 * (qb + 1)
        qT, oT, zT = X4[0], X4[1], X4[2]
        qmT, omT = X4[2], X4[1]
        maskT = hT

        item(lambda s, k: prenorm("mpre"))
        proj2_norm("wcq", "qng", lambda m: cqT[:, m, :], [("cqT", 0), ("cqT", 1)])
        proj2_norm("wckv", "kvng", lambda m: ckvT[:, m, t0:t0 + TBK], [("ckvT", m, qb) for m in range(2)])

        def f_tok(s, k):
            for tl in range(4):
                for m in range(2):
                    tr(psT[:, (tl % 2) * 256 + m * 128:(tl % 2) * 256 + (m + 1) * 128],
                       ckvT[:, m, t0 + tl * 128: t0 + (tl + 1) * 128], [("ckvT", m, qb)])
                cp(ckvtok[:, 4 * qb + tl, :], psT[:, (tl % 2) * 256:(tl % 2) * 256 + 256], [("ps", 7)],
                   [("ckvtok", 4 * qb + tl)])
        item(f_tok)

        def f_kidx(s, k):
            kv = s[:, 0:1024].rearrange("p (c m) -> p c m", c=8)
            wv = s[:, 1024:1056].rearrange("p (c m) -> p c m", c=8)
            if "m" in _SKIP:
                return
            for kc in range(8):
                mm(0, psb[0][:], kv[:, kc, :], hn[:, kc, :], kc == 0, kc == 7, [k, ("hn", kc)])
            if "c" in _SKIP:
                return
            cp(tmpf[:, 0, :], psb[0][:], [("ps", 0)], [("tmpf", 0)], eng="act")
            cp(sq[:, 0, :], tmpf[:, 0, :], [("tmpf", 0)], [("sq", 0)], eng="dve")
            act(sq[:, 1, :], tmpf[:, 0, :], AF.Square, [("tmpf", 0)], [("sq", 1)])
            mm(1, psb[1][:], onesb[:], sq[:, 0, :], True, True, [("sq", 0), ("onesb",)])
            mm(2, psb[2][:], onesb[:], sq[:, 1, :], True, True, [("sq", 1), ("onesb",)])
            if "l" not in _SKIP:
                ln_apply64(psb[1][0:64, :], ("ps", 1), psb[2][0:64, :], ("ps", 2))
            if "w" in _SKIP:
                return
            for qt in range(4):
                for kc in range(8):
                    mm(3, psb[3][:, qt * 4:(qt + 1) * 4], hn[:, kc, qt * 128:(qt + 1) * 128], wv[:, kc, :],
                       kc == 0, kc == 7, [k, ("hn", kc)])
            tsc(widx[:].rearrange("p a b -> p (a b)"), psb[3][:, 0:16], 0.5, ALU.mult, [("ps", 3)], [("widx",)])
        item(f_kidx, "wkidx", 0)

        def ln_apply64(psm, kmean, psv, kvar):
            P_ = slice(0, 64)
            act(tmpf[P_, 1, :], psm, AF.Copy, [kmean], [("tmpf", 1)], scale=1.0 / 64)
            tt(lnt[P_, :], tmpf[P_, 1, :], tmpf[P_, 1, :], ALU.mult, [("tmpf", 1)], [("lnt",)])
            stt(lnt[P_, :], psv, 1.0 / 64, lnt[P_, :], ALU.mult, ALU.subtract, [kvar, ("lnt",)], [("lnt",)])
            act(lnt[P_, :], lnt[P_, :], AF.Ln, [("lnt",), ("epsc",)], [("lnt",)], bias=epsc[P_, :])
            act(rstd[P_, :], lnt[P_, :], AF.Exp, [("lnt",)], [("rstd",)], scale=-0.5)
            tt(tmpf[P_, 0, :], tmpf[P_, 0, :], tmpf[P_, 1, :], ALU.subtract, [("tmpf", 0), ("tmpf", 1)], [("tmpf", 0)])
            stt(tmpf[P_, 0, :], tmpf[P_, 0, :], cst[P_, CO["ilng"]:CO["ilng"] + 1], rstd[P_, :], ALU.mult, ALU.mult,
                [("tmpf", 0), ("cst",), ("rstd",)], [("tmpf", 0)])
            tsc(kidxT[P_, t0:t0 + TBK], tmpf[P_, 0, :], cst[P_, CO["ilnb"]:CO["ilnb"] + 1], ALU.add,
                [("tmpf", 0), ("cst",)], [("kidxT", qb)])

        def f_q(s, k):
            sv = s[:, 0:1024].rearrange("p (m c n) -> p m c n", m=4, c=2)
            for m in range(4):
                b = m % 2
                for kc in range(2):
                    mm(b, psb[b][:], sv[:, m, kc, :], cqT[:, kc, :], kc == 0, kc == 1, [k, ("cqT", kc)])
                cp(qT[:, m, :], psb[b][:], [("ps", b)], [("x40", m)], eng="act" if m % 2 else "dve")
        item(f_q, "wuq", 0)

        def f_qi(s, k):
            sv = s[:, 0:1024].rearrange("p (h c n) -> p h c n", h=4, c=2)
            for h in range(4):
                b = 2 + h % 2
                for kc in range(2):
                    mm(b, psb[b][:], sv[:, h, kc, :], cqT[:, kc, :], kc == 0, kc == 1, [k, ("cqT", kc)])
                act(qidxT[:, h, :], psb[b][:], AF.Copy, [("ps", b)], [("qidxT", h)], scale=0.125)
        item(f_qi, "widxq", 0)

        def sck(qt):
            return [("SC", qt % 2, kb) for kb in range(qb + 1)]

        def scores(qt):
            sl = qt % 2
            gq = 4 * qb + qt
            SCq = SCs[sl]
            for h in range(4):
                tsc(wdiag[:, sl, h, :], identb[:], widx[:, qt, h:h + 1], ALU.mult, [("identb",), ("widx",)],
                    [("wdiag", sl)], eng="pool", s2=0.0, op1=ALU.add)
            for kb in range(qb + 1):
                nk = TBK if kb < qb else (qt + 1) * 128
                c0 = kb * TBK
                pb = 5 + (kb % 2)
                for h in range(4):
                    mm(h, psb[h][:, :nk], qidxT[:, h, qt * 128:(qt + 1) * 128], kidxT[:, c0:c0 + nk], True, True,
                       [("qidxT", h), ("kidxT", kb)])
                    if h % 2 == 0:
                        act(rl[:, h, :nk], psb[h][:, :nk], AF.Relu, [("ps", h)], [("rl", h)])
                    else:
                        tsc(rl[:, h, :nk], psb[h][:, :nk], 0.0, ALU.max, [("ps", h)], [("rl", h)])
                for h in range(4):
                    mm(pb, psb[pb][:, :nk], wdiag[:, sl, h, :], rl[:, h, :nk], h == 0, h == 3,
                       [("wdiag", sl), ("rl", h)])
                if kb < qb:
                    cp(SCq[:, c0:c0 + nk], psb[pb][:, :nk], [("ps", pb)], [("SC", sl, kb)], eng="dve")
                else:
                    if nk > 128:
                        cp(SCq[:, c0:c0 + nk - 128], psb[pb][:, :nk - 128], [("ps", pb)], [("SC", sl, kb)], eng="dve")
                    tt(SCq[:, c0 + nk - 128:c0 + nk], psb[pb][:, nk - 128:nk], causal[:], ALU.add,
                       [("ps", pb), ("causal",)], [("SC", sl, kb)])

        def bisect_gen(qts):
            tiles = [(qt % 2, 4 * qb + qt, (4 * qb + qt + 1) * 128, qt) for qt in qts]
            active = [t for t in tiles if t[1] >= GQ0]
            for (sl, gq, ncols, qt) in active:
                S.add("dve", lambda e, n=gq * 128, sl=sl: e.tensor_reduce(blo[:, sl, 0:1], SCs[sl][:, 0:n], AX.X, ALU.min),
                      reads=sck(qt), writes=[("blo", sl)])
                S.add("dve", lambda e, n=ncols, sl=sl: e.tensor_reduce(blo[:, sl, 1:2], SCs[sl][:, 0:n], AX.X, ALU.max),
                      reads=sck(qt), writes=[("blo", sl)])
            for (sl, gq, ncols, qt) in active:
                tt(bw0[:, sl:sl + 1], blo[:, sl, 1:2], blo[:, sl, 0:1], ALU.subtract, [("blo", sl)], [("bw0", sl)])
            for (sl, gq, ncols, qt) in active:
                tsc(BH[:, sl, :], C("pw", 0, NBIS), bw0[:, sl:sl + 1], ALU.mult, [("cst",), ("bw0", sl)], [("BH", sl)])
                tsc(bmid[:, sl:sl + 1], bw0[:, sl:sl + 1], 0.5, ALU.mult, [("bw0", sl), ("blo", sl)], [("bmid", sl)],
                    s2=blo[:, sl, 0:1], op1=ALU.add)
            for (sl, gq, ncols, qt) in active:
                tsc(BH2[:, sl, :], BH[:, sl, :], 2.0, ALU.mult, [("BH", sl)], [("BH2", sl)])
            yield
            for it in range(NBIS):
                for (sl, gq, ncols, qt) in active:
                    tsc(maskAs[sl][:, :ncols], SCs[sl][:, :ncols], bmid[:, sl:sl + 1], ALU.is_ge,
                        sck(qt) + [("bmid", sl)], [("maskA", sl), ("bcnt", sl)], s2=-(TOPK - 0.5), op1=ALU.add,
                        accum=bcnt[:, sl:sl + 1])
                for (sl, gq, ncols, qt) in active:
                    tsc(bu[:, sl:sl + 1], bcnt[:, sl:sl + 1], 0.0, ALU.is_gt, [("bcnt", sl), ("BH2", sl)], [("bu", sl)],
                        s2=BH2[:, sl, it:it + 1], op1=ALU.mult)
                for (sl, gq, ncols, qt) in active:
                    hh = BH if it < NBIS - 1 else BH2
                    stt(bmid[:, sl:sl + 1], bmid[:, sl:sl + 1], hh[:, sl, it:it + 1], bu[:, sl:sl + 1], ALU.subtract,
                        ALU.add, [("bmid", sl), ("BH", sl), ("BH2", sl), ("bu", sl)], [("bmid", sl)])
                yield
            for (sl, gq, ncols, qt) in tiles:
                if gq >= GQ0:
                    tsc(maskAs[sl][:, :ncols], SCs[sl][:, :ncols], bmid[:, sl:sl + 1], ALU.is_ge,
                        sck(qt) + [("bmid", sl)], [("maskA", sl)])
                else:
                    tsc(maskAs[sl][:, :ncols], SCs[sl][:, :ncols], -1e29, ALU.is_ge, sck(qt), [("maskA", sl)])

        bg = [None]

        def bgstart(qts):
            bg[0] = bisect_gen(qts)

        def bgstep(n=1):
            for _ in range(n):
                if bg[0] is None:
                    return
                try:
                    next(bg[0])
                except StopIteration:
                    bg[0] = None

        def bgdrain():
            while bg[0] is not None:
                bgstep()

        def mask_transposes(qt):
            sl = qt % 2
            gq = 4 * qb + qt
            for k0 in range(0, gq + 1, 4):
                n4 = min(4, gq + 1 - k0)
                half = ((k0 // 4) % 2) * 512
                for i in range(n4):
                    tr(psT[:, half + i * 128: half + (i + 1) * 128], maskAs[sl][:, (k0 + i) * 128:(k0 + i + 1) * 128],
                       [("maskA", sl)])
                cp(maskT[:, k0:k0 + n4, qt * 128:(qt + 1) * 128],
                   psT[:, half:half + n4 * 128].rearrange("p (a b) -> p a b", a=n4), [("ps", 7)],
                   [("hT", k0 + i) for i in range(n4)], eng="act")
            for kc in range(gq + 1, nkc):
                memset(maskT[:, kc, qt * 128:(qt + 1) * 128], 0.0, [("hT", kc)], eng="pool")

        def f_idx01(s, k):
            bgstep_ref[0] = bgstep
            scores(0)
            scores(1)
            bgstart([0, 1])
        item(f_idx01)

        for c in range(4):
            def f(s, k, c=c):
                sv = s[:, 0:2048].rearrange("p (s c m) -> p s c m", s=2, c=8)
                b = gi[0] % 2
                gi[0] += 1
                for kc in range(8):
                    mm(b, psb[b][:], sv[:, 0, kc, :], hn[:, kc, :], kc == 0, kc == 7, [k, ("hn", kc)])
                for kc in range(8):
                    mm(2 + b, psb[2 + b][:], sv[:, 1, kc, :], hn[:, kc, :], kc == 0, kc == 7, [k, ("hn", kc)])
                act(sg[:, b, :], psb[2 + b][:], AF.Sigmoid, [("ps", 2 + b)], [("sg", b)])
                tt(uT[:, c, 32:32 + TBK], psb[b][:], sg[:, b, :], ALU.mult, [("ps", b), ("sg", b)], [("uT", c)])
                bgstep(1)
            item(f, "wu", c)

        def f_conv(s, k):
            for c in range(4):
                for j in range(31):
                    if j % 2 == 0 or "d" in _SKIP:
                        tsc(cdiag[:, j, :], identb[:], C("wdw", j * 4 + c), ALU.mult, [("identb",), ("cst",)],
                            [("cdiag", j)], eng="pool", s2=0.0, op1=ALU.add)
                    else:
                        act(cdiag[:, j, :], identb[:], AF.Identity, [("identb",), ("cst",)], [("cdiag", j)],
                            scale=C("wdw", j * 4 + c))
                b = 5 + c % 2
                for j in range(31):
                    mm(b, psb[b][:], cdiag[:, j, :], uT[:, c, 2 + j:2 + j + TBK], j == 0, j == 30,
                       [("cdiag", j), ("uT", c)])
                cp(uT[:, c, 0:32], uT[:, c, TBK:TBK + 32], [("uT", c)], [("uT", c)], eng="pool")
                act(zT[:, c, :], psb[b][:], AF.Identity, [("ps", b), ("cst",)], [("x42", c)], bias=C("bdw", c))
                q = sqi[0] % 2
                sqi[0] += 1
                act(sq[:, q, :], zT[:, c, :], AF.Square, [("x42", c)], [("sq", q)])
                mm(0, psb[0][:], onesb[:], zT[:, c, :], c == 0, c == 3, [("x42", c), ("onesb",)])
                mm(1, psb[1][:], onesb[:], sq[:, q, :], c == 0, c == 3, [("sq", q), ("onesb",)])
                bgstep(2)
            act(tmpf[:, 1, :], psb[0][:], AF.Copy, [("ps", 0)], [("tmpf", 1)], scale=1.0 / 512)
            tt(lnt[:], tmpf[:, 1, :], tmpf[:, 1, :], ALU.mult, [("tmpf", 1)], [("lnt",)])
            stt(lnt[:], psb[1][:], 1.0 / 512, lnt[:], ALU.mult, ALU.subtract, [("ps", 1), ("lnt",)], [("lnt",)])
            act(lnt[:], lnt[:], AF.Ln, [("lnt",), ("epsc",)], [("lnt",)], bias=epsc[:])
            act(rstd[:], lnt[:], AF.Exp, [("lnt",)], [("rstd",)], scale=-0.5)
            for c in range(4):
                tt(tmpf[:, 0, :], zT[:, c, :], tmpf[:, 1, :], ALU.subtract, [("x42", c), ("tmpf", 1)],
                   [("tmpf", 0)])
                stt(tmpf[:, 0, :], tmpf[:, 0, :], C("clng", c), rstd[:], ALU.mult, ALU.mult,
                    [("tmpf", 0), ("cst",), ("rstd",)], [("tmpf", 0)])
                act(zT[:, c, :], tmpf[:, 0, :], AF.Silu, [("tmpf", 0), ("cst",)], [("x42", c)], bias=C("clnb", c))
        item(f_conv)
        gated_out("wgb", zT, "x42", True, nbg=1)

        def f_idx23(s, k):
            bgdrain()
            mask_transposes(0)
            mask_transposes(1)
            scores(2)
            scores(3)
            bgstart([2, 3])
        item(f_idx23)

        for g in range(2):
            def f(s, k, g=g):
                sv = s[:, 0:2048].rearrange("p (s c m) -> p s c m", s=2, c=8)
                for m in range(2):
                    b = m
                    for kc in range(8):
                        mm(b, psb[b][:], sv[:, m, kc, :], hn[:, kc, :], kc == 0, kc == 7, [k, ("hn", kc)])
                    cp(qmT[:, 2 * g + m, :], psb[b][:], [("ps", b)], [("x42", 2 * g + m)], eng="act")
                bgstep(2)
            item(f, "wqm", g)

        pti = [0]

        def f_mattn(s, k):
            for h in range(4):
                hb = h % 2
                for mc2 in range(2):
                    mm(mc2, psb[mc2][:], memkT[:, h, mc2 * 128:(mc2 + 1) * 128], qmT[:, h, :], True, True,
                       [("memkT", h), ("x42", h)])
                    p = pti[0] % 3
                    pti[0] += 1
                    act(PT[:, p, :], psb[mc2][:], AF.Exp, [("ps", mc2)], [("PT", p)], scale=128.0 ** -0.5)
                    mm(2 + hb, psb[2 + hb][:], memv[:, mc2, h * 128:(h + 1) * 128], PT[:, p, :], mc2 == 0, mc2 == 1,
                       [("memv", mc2), ("PT", p)])
                    mm(5 + hb, psb[5 + hb][:], onesb[:], PT[:, p, :], mc2 == 0, mc2 == 1, [("onesb",), ("PT", p)])
                act(lnt[:], psb[5 + hb][:], AF.Ln, [("ps", 5 + hb)], [("lnt",)])
                act(rinv[:, hb, :], lnt[:], AF.Exp, [("lnt",)], [("rinv", hb)], scale=-1.0)
                tt(omT[:, h, :], psb[2 + hb][:], rinv[:, hb, :], ALU.mult, [("ps", 2 + hb), ("rinv", hb)], [("x41", h)])
                bgstep(1)
        item(f_mattn)
        gated_out("wgm", omT, "x41", False, nbg=2)

        def f_idx_end(s, k):
            bgdrain()
            mask_transposes(2)
            mask_transposes(3)
        item(f_idx_end)

        def f_attn(s, k):
            steps = [(h, kc) for h in range(8) for kc in range(nkc)]

            def emit_qlat(h, ccs=(0, 1)):
                hb = h % 2
                for cc in ccs:
                    mm(4, psb[4][:], wukr[:, h, cc, :], qT[:, h // 2, :], True, True, [("wukr",), ("x40", h // 2)])
                    act(qlat[:, hb, cc, :], psb[4][:], AF.Copy, [("ps", 4)], [("qlat", hb, cc)], scale=0.125)

            def q0(kc):
                return max(0, kc - 4 * qb) * 128

            def emit_S(i):
                h, kc = steps[i]
                hb, sbk, p = h % 2, i % 2, i % 3
                c0 = q0(kc)
                for cc in range(2):
                    mm(sbk, psb[sbk][:, c0:], ckvT[:, cc, kc * 128:(kc + 1) * 128], qlat[:, hb, cc, c0:], cc == 0,
                       cc == 1, [("ckvT", cc, kc // 4), ("qlat", hb, cc)])
                act(PT[:, p, c0:], psb[sbk][:, c0:], AF.Exp, [("ps", sbk)], [("PT", p)])
                tt(PT[:, p, c0:], PT[:, p, c0:], maskT[:, kc, c0:], ALU.mult, [("PT", p), ("hT", kc)], [("PT", p)])

            def emit_PV(i):
                h, kc = steps[i]
                p = i % 3
                c0 = q0(kc)
                for cc in range(2):
                    mm(2 + cc, psb[2 + cc][:, c0:], ckvtok[:, kc, cc * 128:(cc + 1) * 128], PT[:, p, c0:], kc == 0,
                       kc == nkc - 1, [("ckvtok", kc), ("PT", p)])
                mm(5, psb[5][:, c0:], onesb[:], PT[:, p, c0:], kc == 0, kc == nkc - 1, [("onesb",), ("PT", p)])

            def fin_elem(h):
                hb = h % 2
                act(lnt[:], psb[5][:], AF.Ln, [("ps", 5)], [("lnt",)])
                act(rinv[:, hb, :], lnt[:], AF.Exp, [("lnt",)], [("rinv", hb)], scale=-1.0)
                for cc in range(2):
                    tt(olat[:, hb, cc, :], psb[2 + cc][:], rinv[:, hb, :], ALU.mult, [("ps", 2 + cc), ("rinv", hb)],
                       [("olat", hb, cc)])

            def fin_pe(h):
                hb = h % 2
                for cc in range(2):
                    mm(6, psb[6][:], wuvr[:, h, cc, :], olat[:, hb, cc, :], hb == 0 and cc == 0, hb == 1 and cc == 1,
                       [("wuvr",), ("olat", hb, cc)])
                if hb == 1:
                    cp(oT[:, h // 2, :], psb[6][:], [("ps", 6)], [("x41", h // 2)], eng="act")

            emit_qlat(0)
            emit_S(0)
            pend = []
            for i in range(len(steps)):
                h, kc = steps[i]
                if kc < 2 and h + 1 < 8:
                    emit_qlat(h + 1, (kc,))
                if "p" in _SKIP:
                    if i > 0:
                        emit_S(i)
                elif i + 1 < len(steps):
                    emit_S(i + 1)
                emit_PV(i)
                for ph in pend:
                    fin_pe(ph)
                pend = []
                if kc == nkc - 1:
                    fin_elem(h)
                    pend.append(h)
            for ph in pend:
                fin_pe(ph)
        item(f_attn)
        gated_out("wga", oT, "x41", False, nbg=0)

        def f_mb(s, k):
            for kc in range(8):
                cp(hn[:, kc, :], yb[:, kc, :], [("yb", kc)], [("hn", kc)], eng="act" if kc % 2 else "dve")
        item(f_mb)
        for g in range(4):
            def f(s, k, g=g):
                sv = s[:, 0:2048].rearrange("p (s c m) -> p s c m", s=2, c=8)
                for m in range(2):
                    mc = 2 * g + m
                    b = 5 + m
                    for kc in range(8):
                        mm(b, psb[b][:], sv[:, m, kc, :], hn[:, kc, :], kc == 0, kc == 7, [k, ("hn", kc)])
                    flush_stats()
                    q = sqi[0] % 2
                    sqi[0] += 1
                    act(sq[:, q, :], psb[b][:], AF.Square, [("ps", b)], [("sq", q)])
                    cp(yb[:, mc, :], psb[b][:], [("ps", b)], [("yb", mc)], eng="act")
                    pend_stats.append((q, mc))
            item(f, "wout", g)

        def f_mpost(s, k):
            flush_stats()
            postnorm_residual(lambda mc: C("mpost", mc), ("cst",))
        item(f_mpost)

    def mem_prep(seq):
        sem = new_dsem()

        def f_load(s, k):
            for kc in range(8):
                dma_plain(SCs[0][:, kc * NMEM:(kc + 1) * NMEM], memT[seq, kc * 128:(kc + 1) * 128, :], [],
                          [("SC", 0, kc // 2)], sem)
            rms_stats([SCs[0][:, kc * NMEM:(kc + 1) * NMEM] for kc in range(8)], [("SC", 0, kc // 2) for kc in range(8)],
                      NMEM, 1.0 / D)
            mn = X4[2][:].rearrange("p a b -> p (a b)")
            for kc in range(8):
                stt(mn[:, kc * NMEM:(kc + 1) * NMEM], SCs[0][:, kc * NMEM:(kc + 1) * NMEM], C("memg", kc),
                    rstd[:, :NMEM], ALU.mult, ALU.mult, [("SC", 0, kc // 2), ("cst",), ("rstd",)], [("x42", kc // 2)])
        item(f_load)
        for g in range(2):
            def f(s, k, g=g):
                sv = s[:, 0:2048].rearrange("p (s c m) -> p s c m", s=2, c=8)
                mn = X4[2][:].rearrange("p a b -> p (a b)")
                for m in range(2):
                    b = m
                    for kc in range(8):
                        mm(b, psb[b][:, :NMEM], sv[:, m, kc, :], mn[:, kc * NMEM:(kc + 1) * NMEM], kc == 0, kc == 7,
                           [k, ("x42", kc // 2)])
                    cp(memkT[:, 2 * g + m, :], psb[b][:, :NMEM], [("ps", b)], [("memkT", 2 * g + m)],
                       eng="act" if m else "dve")
            item(f, "wmk", g)
        for g in range(2):
            def f(s, k, g=g):
                sv = s[:, 0:2048].rearrange("p (c n) -> p c n", c=4)
                mn = X4[2][:].rearrange("p a b -> p (a b)")
                for mc2 in range(2):
                    for kl in range(4):
                        kc = 4 * g + kl
                        mm(2 + mc2, psb[2 + mc2][:], mn[:, kc * NMEM + mc2 * 128: kc * NMEM + (mc2 + 1) * 128],
                           sv[:, kl, :], kc == 0, kc == 7, [k, ("x42", kc // 2)])
                    if g == 1:
                        cp(memv[:, mc2, :], psb[2 + mc2][:], [("ps", 2 + mc2)], [("memv", mc2)],
                           eng="act" if mc2 else "dve")
            item(f, "wmv", g)

    out_sems = []
    for seq in range(NSEQ):
        mem_prep(seq)
        if seq > 0:
            item(lambda s, k: memset(uT[:], 0.0, [("uT", c) for c in range(4)], eng="dve"))
        for qb in range(NQB):
            t0 = qb * TBK
            semx = new_dsem()

            def f_x(s, k, seq=seq, t0=t0, semx=semx):
                for kc in range(8):
                    dma_plain(xres[:, kc, :], xT[seq, kc * 128:(kc + 1) * 128, t0:t0 + TBK], [], [("xres", kc)], semx)
            item(f_x)
            ffn("w1gu", "w1dn", "f1pre", 0)
            mixer(seq, qb)
            ffn("w2gu", "w2dn", "f2pre", 8)
            semo = new_dsem()
            out_sems.append(semo)

            def f_o(s, k, seq=seq, t0=t0, semo=semo):
                for kc in range(8):
                    dma_plain(outT[seq, kc * 128:(kc + 1) * 128, t0:t0 + TBK], xres[:, kc, :], [("xres", kc)],
                              [("out", seq, t0, kc)], semo)
            item(f_o)

    loads = [(i, it) for i, it in enumerate(items) if it[0] is not None]
    slot_of = {}
    nl = [0]

    def issue_load():
        li = nl[0]
        if li >= len(loads):
            return
        i, (w, g, _) = loads[li]
        sl = li % NBUF
        n = weight_specs()[w][1]
        dma_cast(ring[sl][:, 0:n], wd[w][g], [], [("ring", sl)], ring_sem[sl])
        slot_of[i] = sl
        nl[0] += 1

    import os as _os
    _n = int(_os.environ.get("KDBG_N", "0"))
    if _n:
        items[:] = items[:_n]
        loads[:] = [(i, it) for i, it in enumerate(items) if it[0] is not None]
    for _ in range(NBUF - 1):
        issue_load()
    for i, (w, g, fn) in enumerate(items):
        if w is not None:
            issue_load()
            sl = slot_of[i]
            fn(ring[sl], ("ring", sl))
        else:
            fn(None, None)

    fin_reads = []
    for seq in range(NSEQ):
        for qb in range(NQB):
            for kc in range(8):
                fin_reads.append(("out", seq, qb * TBK, kc))
    S.add("sp", lambda e: e.nop(), reads=fin_reads, writes=[])

    S.finalize()
    with ExitStack() as es2:
        for k in set(op.skey for op in S.ops if op.sig):
            sems[k] = es2.enter_context(nc.semaphore("s_%s_%s" % (k[0], k[1])))
        block = es2.enter_context(nc.Block())

        @block.tensor
        def _(e):
            S.emit("pe", e, sems)

        @block.scalar
        def _(e):
            S.emit("act", e, sems)

        @block.vector
        def _(e):
            S.emit("dve", e, sems)

        @block.gpsimd
        def _(e):
            S.emit("pool", e, sems)

        @block.sync
        def _(e):
            S.emit("sp", e, sems)
    es.close()
    return nc


def lhsT_tiles(W, mchunk=128):
    K, M = W.shape
    return np.ascontiguousarray(W.reshape(K // 128, 128, M // mchunk, mchunk).transpose(2, 1, 0, 3))


def col_tile(v):
    n = v.shape[0]
    if n < 128:
        o = np.zeros((128, 1), np.float32)
        o[:n, 0] = v
        return o
    return np.ascontiguousarray(v.reshape(n // 128, 128).T)


def prep_weights(p):
    f = np.float32
    w = {}
    for i, gu, dn in ((1, p["ffn1_w_gu"][0], p["ffn1_w_down"][0]), (2, p["ffn2_w_gu"][0], p["ffn2_w_down"][0])):
        g = lhsT_tiles(gu[:, :DFF])
        u = lhsT_tiles(gu[:, DFF:])
        w["w%dgu" % i] = np.stack([g, u], axis=2).reshape(NJ, 128, 2048)
        w["w%ddn" % i] = lhsT_tiles(dn).reshape(8, 128, DFF)
    win = p["w_in"][0]
    o_cq, o_ckv, o_ki, o_wi, o_u, o_qm, o_g = 0, 256, 512, 576, 580, 1604, 2116
    w["wcq"] = lhsT_tiles(win[:, o_cq:o_cq + 256]).transpose(1, 0, 2, 3).reshape(1, 128, 2048)
    w["wckv"] = lhsT_tiles(win[:, o_ckv:o_ckv + 256]).transpose(1, 0, 2, 3).reshape(1, 128, 2048)
    kip = np.zeros((D, 128), f)
    kip[:, :64] = win[:, o_ki:o_ki + 64]
    ki = lhsT_tiles(kip)[0].reshape(128, 1024)
    wi = lhsT_tiles(win[:, o_wi:o_wi + 4], 4)[0].reshape(128, 32)
    w["wkidx"] = np.concatenate([ki, wi], axis=1)[None]
    w["wuq"] = lhsT_tiles(p["w_uq"][0]).transpose(1, 0, 2, 3).reshape(1, 128, 1024)
    wiq = np.zeros((256, 4, 128), f)
    wiq[:, :, :64] = p["w_idx_q"][0].reshape(256, 4, 64)
    w["widxq"] = lhsT_tiles(wiq.reshape(256, 512)).transpose(1, 0, 2, 3).reshape(1, 128, 1024)
    wuk = p["w_uk"][0]
    wuv = p["w_uv"][0]
    uk = np.zeros((128, 8, 2, 128), f)
    uv = np.zeros((128, 8, 2, 128), f)
    for h in range(8):
        r0 = (h % 2) * 64
        for cc in range(2):
            uk[r0:r0 + 64, h, cc, :] = wuk[cc * 128:(cc + 1) * 128, h, :].T
            uv[:, h, cc, r0:r0 + 64] = wuv[cc * 128:(cc + 1) * 128, h, :]
    w["wuk"] = uk.reshape(1, 128, 2048)
    w["wuv"] = uv.reshape(1, 128, 2048)
    for bi, (nm, wo) in enumerate((("wga", "w_dsa_o"), ("wgb", "w_conv_out"), ("wgm", "w_mem_o"))):
        gt = lhsT_tiles(win[:, o_g + bi * D:o_g + (bi + 1) * D]).reshape(8, 128, 1024)
        ot = lhsT_tiles(p[wo][0]).reshape(8, 128, 512)
        w[nm] = np.concatenate([gt, ot], axis=2)
    ua = lhsT_tiles(win[:, o_u:o_u + 512])
    ug = lhsT_tiles(win[:, o_u + 512:o_u + 1024])
    w["wu"] = np.stack([ua, ug], axis=2).reshape(4, 128, 2048)
    qm = lhsT_tiles(win[:, o_qm:o_qm + 512])
    w["wqm"] = qm.reshape(2, 2, 128, 8, 128).transpose(0, 2, 1, 3, 4).reshape(2, 128, 2048)
    mkv = p["w_mem_kv"][0]
    mk = lhsT_tiles(mkv[:, :512])
    w["wmk"] = mk.reshape(2, 2, 128, 8, 128).transpose(0, 2, 1, 3, 4).reshape(2, 128, 2048)
    mv = mkv[:, 512:].reshape(8, 128, 512)
    w["wmv"] = mv.reshape(2, 4, 128, 512).transpose(0, 2, 1, 3).reshape(2, 128, 2048)
    wo = lhsT_tiles(p["w_out"][0])
    w["wout"] = wo.reshape(4, 2, 128, 8, 128).transpose(0, 2, 1, 3, 4).reshape(4, 128, 2048)
    CO, NC_ = cst_layout()
    cst = np.zeros((128, NC_), f)

    def put(name, v):
        t = col_tile(np.asarray(v, f))
        cst[:, CO[name]:CO[name] + t.shape[1]] = t
    put("f1pre", p["ffn1_pre_g"][0]); put("f1post", p["ffn1_post_g"][0])
    put("mpre", p["mix_pre_g"][0]); put("mpost", p["mix_post_g"][0])
    put("f2pre", p["ffn2_pre_g"][0]); put("f2post", p["ffn2_post_g"][0])
    put("memg", p["mem_norm_g"][0]); put("qng", p["q_norm_g"][0]); put("kvng", p["kv_norm_g"][0])
    put("ilng", p["idx_ln_g"][0]); put("ilnb", p["idx_ln_b"][0])
    put("bdw", p["b_dw"][0]); put("clng", p["conv_ln_g"][0]); put("clnb", p["conv_ln_b"][0])
    wdw = p["w_dw"][0]
    cst[:, CO["wdw"]:CO["wdw"] + 124] = wdw.reshape(31, 4, 128).transpose(2, 0, 1).reshape(128, 124)
    cst[:, CO["pw"]:CO["pw"] + NBIS] = np.float32(2.0) ** -(np.arange(NBIS, dtype=f) + 2)
    w["cst"] = cst
    w["ident"] = np.eye(128, dtype=f)
    cm = np.zeros((128, 128), f)
    cm[np.triu_indices(128, 1)] = -1e30
    w["causal"] = cm
    return {k: np.ascontiguousarray(v, dtype=f) for k, v in w.items()}


_NC_CACHE = {}


def run(inputs, T, B, n_cores, runner=None):
    p = {k: np.asarray(v) for k, v in inputs.items()}
    NSEQ = B // n_cores
    w = prep_weights(p)
    x = np.asarray(p["x"], np.float32)
    mem = np.asarray(p["mem"], np.float32)
    xT = np.ascontiguousarray(x.transpose(0, 2, 1))
    mT = np.ascontiguousarray(mem.transpose(0, 2, 1))
    key = (T, NSEQ)
    if key not in _NC_CACHE:
        _NC_CACHE[key] = build_nc(T, NSEQ)
    nc = _NC_CACHE[key]
    in_maps = []
    for c in range(n_cores):
        m = dict(w)
        m["xT"] = xT[c * NSEQ:(c + 1) * NSEQ]
        m["memT"] = mT[c * NSEQ:(c + 1) * NSEQ]
        in_maps.append(m)
    if runner is None:
        res = run_bass_kernel_spmd(nc, in_maps, core_ids=list(range(n_cores)))
        outs = [r["outT"] for r in res.results]
    else:
        outs = runner(nc, in_maps)
    oT = np.concatenate(outs, axis=0)
    return np.ascontiguousarray(oT.transpose(0, 2, 1)).astype(np.float32)


def kernel(**inputs):
    return run(inputs, 2048, 16, 8)
```

```python
import numpy as np
from contextlib import ExitStack
import concourse.bass as bass
import concourse.mybir as mybir
from concourse.bass_utils import run_bass_kernel_spmd

F32 = mybir.dt.float32
BF16 = mybir.dt.bfloat16
AF = mybir.ActivationFunctionType
ALU = mybir.AluOpType
AX = mybir.AxisListType

D = 1024
DFF = 2816
NJ = DFF // 128
NMEM = 256
EPS = 1e-6
NBIS = 14
SLOT = 2816
import os as _os0
_SKIP = _os0.environ.get("KDBG_SKIP", "")
NBUF = 4
TBK = 512


class Op:
    __slots__ = ("eng", "fn", "deps", "sig", "cnt", "dsem", "idx", "skey", "wcnt")


class Sched:
    ENGS = ("pe", "act", "dve", "pool", "sp")

    def __init__(self):
        self.ops = []
        self.lastw = {}
        self.readers = {}
        self.ndsem = 0

    def add(self, eng, fn, reads=(), writes=(), dsem=None):
        op = Op()
        op.eng, op.fn, op.idx, op.sig, op.cnt, op.dsem = eng, fn, len(self.ops), False, 0, dsem
        deps = set()
        for k in reads:
            w = self.lastw.get(k)
            if w is not None:
                deps.add(w)
        for k in writes:
            w = self.lastw.get(k)
            if w is not None:
                deps.add(w)
            r = self.readers.get(k)
            if r:
                deps.update(r.values())
        for k in reads:
            self.readers.setdefault(k, {})[(eng, dsem if dsem is not None else -1, op.idx if dsem is not None else 0)] = op.idx
        for k in writes:
            self.lastw[k] = op.idx
            self.readers[k] = {}
        op.deps = deps
        self.ops.append(op)
        return op

    def finalize(self):
        ops = self.ops
        for op in ops:
            keep = set()
            for d in op.deps:
                p = ops[d]
                if p.dsem is None and p.eng == "pe" and op.eng == "pe" and op.dsem is None:
                    continue
                if p.dsem is not None and p.dsem == op.dsem:
                    continue
                keep.add(d)
                p.sig = True
            op.deps = keep
        cnt = {}
        for op in ops:
            if op.dsem is not None:
                op.sig = True
            if not op.sig:
                continue
            key = ("d", op.dsem) if op.dsem is not None else ("e", op.eng)
            cnt[key] = cnt.get(key, 0) + (16 if op.dsem is not None else 1)
            op.cnt = cnt[key]
            op.wcnt = op.cnt
            op.skey = key
        i = len(ops) - 1
        while i >= 0:
            op = ops[i]
            if op.dsem is not None:
                j = i
                while j - 1 >= 0 and ops[j - 1].dsem == op.dsem:
                    j -= 1
                for t in range(j, i + 1):
                    ops[t].wcnt = op.cnt
                i = j - 1
            else:
                i -= 1

    def emit(self, eng_name, eng, sems):
        waited = {}
        ops = self.ops
        for op in ops:
            if op.eng != eng_name:
                continue
            need = {}
            for d in op.deps:
                p = ops[d]
                k = p.skey
                if p.wcnt > need.get(k, 0):
                    need[k] = p.wcnt
            for k, v in need.items():
                if v > waited.get(k, 0):
                    eng.wait_ge(sems[k], v)
                    waited[k] = v
            ins = op.fn(eng)
            if op.sig:
                ins.then_inc(sems[op.skey], 16 if op.dsem is not None else 1)


def cst_layout():
    names = [("f1pre", 8), ("f1post", 8), ("mpre", 8), ("mpost", 8), ("f2pre", 8), ("f2post", 8), ("memg", 8),
             ("qng", 2), ("kvng", 2), ("ilng", 1), ("ilnb", 1), ("bdw", 4), ("clng", 4), ("clnb", 4), ("wdw", 124), ("pw", NBIS)]
    off = {}
    c = 0
    for n, w in names:
        off[n] = c
        c += w
    return off, c


def weight_specs():
    return {
        "w1gu": (NJ, 2048), "w1dn": (8, DFF), "w2gu": (NJ, 2048), "w2dn": (8, DFF),
        "wcq": (1, 2048), "wckv": (1, 2048), "wkidx": (1, 8 * 128 + 8 * 4),
        "wuq": (1, 1024), "widxq": (1, 1024), "wuk": (1, 2048), "wuv": (1, 2048),
        "wga": (8, 1536), "wgb": (8, 1536), "wgm": (8, 1536),
        "wu": (4, 2048), "wqm": (2, 2048), "wmk": (2, 2048), "wmv": (2, 2048), "wout": (4, 2048),
    }


def build_nc(T, NSEQ):
    NQB = T // TBK
    NKC = T // 128
    TOPK = min(256, T // 4)
    GQ0 = TOPK // 128
    assert TOPK % 128 == 0
    CO, NC_ = cst_layout()

    nc = bass.Bass("TRN2", target_bir_lowering=False)
    S = Sched()
    es = ExitStack()

    def dram(name, shape, kind="ExternalInput"):
        return nc.dram_tensor(name, list(shape), F32, kind=kind).ap()

    xT = dram("xT", [NSEQ, D, T])
    memT = dram("memT", [NSEQ, D, NMEM])
    cstd = dram("cst", [128, NC_])
    identd = dram("ident", [128, 128])
    causald = dram("causal", [128, 128])
    wd = {n: dram(n, [g, 128, f]) for n, (g, f) in weight_specs().items()}
    outT = dram("outT", [NSEQ, D, T], kind="ExternalOutput")

    def sb(name, shape, dt):
        return es.enter_context(nc.sbuf_tensor(name, list(shape), dt))

    xres = sb("xres", [128, 8, TBK], F32)
    hn = sb("hn", [128, 8, TBK], BF16)
    hT = sb("hT", [128, NJ, TBK], BF16)
    yb = sb("yb", [128, 8, TBK], F32)
    sq = sb("sq", [128, 2, TBK], BF16)
    rstd = sb("rstd", [128, TBK], F32)
    lnt = sb("lnt", [128, TBK], F32)
    sg = sb("sg", [128, 2, TBK], F32)
    tmpf = sb("tmpf", [128, 2, TBK], F32)
    rinv = sb("rinv", [128, 2, TBK], F32)
    ckvT = sb("ckvT", [128, 2, T], BF16)
    ckvtok = sb("ckvtok", [128, NKC, 256], BF16)
    kidxT = sb("kidxT", [128, T], BF16)
    cqT = sb("cqT", [128, 2, TBK], BF16)
    X4 = [sb("x4%d" % i, [128, 4, TBK], BF16) for i in range(3)]
    qidxT = sb("qidxT", [128, 4, TBK], BF16)
    widx = sb("widx", [128, 4, 4], F32)
    wdiag = sb("wdiag", [128, 2, 4, 128], BF16)
    rl = sb("rl", [128, 4, TBK], BF16)
    SCs = [sb("SC%d" % i, [128, max(T, 2048) if i == 0 else T], F32) for i in range(2)]
    maskAs = [sb("maskA%d" % i, [128, T], BF16) for i in range(2)]
    blo = sb("blo", [128, 2, 2], F32)
    bw0 = sb("bw0", [128, 2], F32)
    BH = sb("BH", [128, 2, NBIS], F32)
    BH2 = sb("BH2", [128, 2, NBIS], F32)
    bmid = sb("bmid", [128, 2], F32)
    bcnt = sb("bcnt", [128, 2], F32)
    bu = sb("bu", [128, 2], F32)
    qlat = sb("qlat", [128, 2, 2, TBK], BF16)
    PT = sb("PT", [128, 3, TBK], BF16)
    olat = sb("olat", [128, 2, 2, TBK], BF16)
    uT = sb("uT", [128, 4, 32 + TBK], BF16)
    cdiag = sb("cdiag", [128, 31, 128], BF16)
    memkT = sb("memkT", [128, 4, NMEM], BF16)
    memv = sb("memv", [128, 2, 512], BF16)
    wukr = sb("wukr", [128, 8, 2, 128], BF16)
    wuvr = sb("wuvr", [128, 8, 2, 128], BF16)
    cst = sb("cstsb", [128, NC_], F32)
    ghalf = sb("ghalf", [128, 16], F32)
    identb = sb("identb", [128, 128], BF16)
    onesb = sb("onesb", [128, 128], BF16)
    causal = sb("causalsb", [128, 128], F32)
    epsc = sb("epsc", [128, 1], F32)
    ring = [sb("ring%d" % i, [128, SLOT], BF16) for i in range(NBUF)]
    psb = [es.enter_context(nc.psum_tensor("ps%d" % i, [128, 512], F32)) for i in range(7)]
    psT = es.enter_context(nc.psum_tensor("psT", [128, 1024], BF16))

    sems = {}

    def new_dsem():
        S.ndsem += 1
        return S.ndsem - 1

    def mm(bank, out, lhsT, rhs, start, stop, reads):
        S.add("pe", lambda e: e.matmul(out, lhsT, rhs, start=start, stop=stop), reads=reads, writes=[("ps", bank)])

    def tr(out, in_, reads):
        S.add("pe", lambda e: e.transpose(out, in_, identb[:]), reads=reads + [("identb",)], writes=[("ps", 7)])

    def act(out, in_, func, reads, writes, scale=1.0, bias=None, eng="act"):
        if bias is None:
            S.add(eng, lambda e: e.activation(out, in_, func, scale=scale), reads=reads, writes=writes)
        else:
            S.add(eng, lambda e: e.activation(out, in_, func, bias=bias, scale=scale), reads=reads, writes=writes)

    def tsc(out, in0, s1, op0, reads, writes, s2=None, op1=None, accum=None, eng="dve"):
        def f(e):
            if op1 is None:
                return e.tensor_scalar(out, in0, s1, None, op0)
            if accum is None:
                return e.tensor_scalar(out, in0, s1, s2, op0, op1)
            return e.tensor_scalar(out, in0, s1, s2, op0, op1, accum_out=accum)
        S.add(eng, f, reads=reads, writes=writes)

    def tt(out, in0, in1, op, reads, writes, eng="dve"):
        S.add(eng, lambda e: e.tensor_tensor(out, in0, in1, op), reads=reads, writes=writes)

    def stt(out, in0, scalar, in1, op0, op1, reads, writes):
        S.add("dve", lambda e: e.scalar_tensor_tensor(out, in0, scalar, in1, op0, op1), reads=reads, writes=writes)

    def cp(out, in_, reads, writes, eng="dve"):
        if eng == "act":
            S.add("act", lambda e: e.copy(out, in_), reads=reads, writes=writes)
        else:
            S.add(eng, lambda e: e.tensor_copy(out, in_), reads=reads, writes=writes)

    def recip(out, in_, reads, writes):
        S.add("dve", lambda e: e.reciprocal(out, in_), reads=reads, writes=writes)

    def memset(ap, v, writes, eng="pool"):
        S.add(eng, lambda e: e.memset(ap, v), writes=writes)

    def dma_plain(out, in_, reads, writes, dsem):
        S.add("sp", lambda e: e.dma_start(out, in_), reads=reads, writes=writes, dsem=dsem)

    def dma_cast(out, in_, reads, writes, dsem):
        S.add("pool", lambda e: e.dma_start(out, in_, max_dma_last_dim=8192), reads=reads, writes=writes, dsem=dsem)

    C = lambda name, i=0, n=1: cst[:, CO[name] + i: CO[name] + i + n]

    items = []

    def item(fn, w=None, g=0):
        items.append((w, g, fn))

    ring_sem = [new_dsem() for _ in range(NBUF)]

    sem_c = new_dsem()
    dma_plain(cst[:], cstd[:], [], [("cst",)], sem_c)
    dma_plain(causal[:], causald[:], [], [("causal",)], sem_c)
    sem_c2 = new_dsem()
    dma_cast(identb[:], identd[:], [], [("identb",)], sem_c2)
    dma_cast(wukr[:].rearrange("p a b c -> p (a b c)"), wd["wuk"][0], [], [("wukr",)], sem_c2)
    dma_cast(wuvr[:].rearrange("p a b c -> p (a b c)"), wd["wuv"][0], [], [("wuvr",)], sem_c2)
    memset(onesb[:], 1.0, [("onesb",)], eng="dve")
    memset(epsc[:], EPS, [("epsc",)], eng="dve")
    memset(uT[:], 0.0, [("uT", c) for c in range(4)], eng="dve")
    memset(kidxT[:], 0.0, [("kidxT", q) for q in range(NQB)], eng="dve")
    tsc(ghalf[:, 0:8], C("f1post", 0, 8), 0.5, ALU.mult, [("cst",)], [("ghalf",)])
    tsc(ghalf[:, 8:16], C("f2post", 0, 8), 0.5, ALU.mult, [("cst",)], [("ghalf",)])

    sqi = [0]

    def rms_stats(src_aps, src_keys, ncols, inv_n):
        n = len(src_aps)
        for i, (ap, k) in enumerate(zip(src_aps, src_keys)):
            b = sqi[0] % 2
            sqi[0] += 1
            act(sq[:, b, :ncols], ap, AF.Square, [k], [("sq", b)])
            mm(4, psb[4][:, :ncols], onesb[:], sq[:, b, :ncols], i == 0, i == n - 1, [("sq", b), ("onesb",)])
        finish_rstd(psb[4][:, :ncols], ("ps", 4), ncols, inv_n)

    def finish_rstd(ps_ap, ps_key, ncols, inv_n):
        act(lnt[:, :ncols], ps_ap, AF.Ln, [ps_key, ("epsc",)], [("lnt",)], scale=inv_n, bias=epsc[:])
        act(rstd[:, :ncols], lnt[:, :ncols], AF.Exp, [("lnt",)], [("rstd",)], scale=-0.5)

    def prenorm(gname):
        rms_stats([xres[:, kc, :] for kc in range(8)], [("xres", kc) for kc in range(8)], TBK, 1.0 / D)
        for kc in range(8):
            stt(hn[:, kc, :], xres[:, kc, :], C(gname, kc), rstd[:], ALU.mult, ALU.mult,
                [("xres", kc), ("cst",), ("rstd",)], [("hn", kc)])

    def postnorm_residual(gap_fn, gkey):
        finish_rstd(psb[4][:], ("ps", 4), TBK, 1.0 / D)
        for mc in range(8):
            b = mc % 2
            stt(tmpf[:, b, :], yb[:, mc, :], gap_fn(mc), rstd[:], ALU.mult, ALU.mult,
                [("yb", mc), gkey, ("rstd",)], [("tmpf", b)])
            tt(xres[:, mc, :], xres[:, mc, :], tmpf[:, b, :], ALU.add, [("xres", mc), ("tmpf", b)], [("xres", mc)],
               eng="pool")

    gi = [0]
    pend_stats = []

    def flush_stats():
        for (q, mc) in pend_stats:
            mm(4, psb[4][:], onesb[:], sq[:, q, :], mc == 0, mc == 7, [("sq", q), ("onesb",)])
        del pend_stats[:]

    def ffn(wgu, wdn, gpre, ghoff):
        item(lambda s, k: prenorm(gpre))
        for j in range(NJ):
            def f(s, k, j=j):
                sv = s[:, 0:2048].rearrange("p (s c m) -> p s c m", s=2, c=8)
                b = gi[0] % 2
                gi[0] += 1
                for kc in range(8):
                    mm(b, psb[b][:], sv[:, 0, kc, :], hn[:, kc, :], kc == 0, kc == 7, [k, ("hn", kc)])
                for kc in range(8):
                    mm(2 + b, psb[2 + b][:], sv[:, 1, kc, :], hn[:, kc, :], kc == 0, kc == 7, [k, ("hn", kc)])
                act(sg[:, b, :], psb[b][:], AF.Silu, [("ps", b)], [("sg", b)])
                tt(hT[:, j, :], psb[2 + b][:], sg[:, b, :], ALU.mult, [("ps", 2 + b), ("sg", b)], [("hT", j)])
            item(f, wgu, j)
        for mc in range(8):
            def f(s, k, mc=mc):
                sv = s[:, 0:DFF].rearrange("p (c m) -> p c m", c=NJ)
                b = 5 + mc % 2
                for kc in range(NJ):
                    mm(b, psb[b][:], sv[:, kc, :], hT[:, kc, :], kc == 0, kc == NJ - 1, [k, ("hT", kc)])
                flush_stats()
                q = sqi[0] % 2
                sqi[0] += 1
                act(sq[:, q, :], psb[b][:], AF.Square, [("ps", b)], [("sq", q)])
                cp(yb[:, mc, :], psb[b][:], [("ps", b)], [("yb", mc)], eng="act")
                pend_stats.append((q, mc))
            item(f, wdn, mc)

        def f_post(s, k):
            flush_stats()
            postnorm_residual(lambda mc: ghalf[:, ghoff + mc: ghoff + mc + 1], ("ghalf",))
        item(f_post)

    def gated_out(wname, rhs_t, rhs_name, first, nbg=0):
        ce = "pool" if nbg else "dve"
        for mc in range(8):
            def f(s, k, mc=mc):
                gv = s[:, 0:1024].rearrange("p (c m) -> p c m", c=8)
                ov = s[:, 1024:1536].rearrange("p (c m) -> p c m", c=4)
                b = gi[0] % 2
                gi[0] += 1
                for kc in range(8):
                    mm(b, psb[b][:], gv[:, kc, :], hn[:, kc, :], kc == 0, kc == 7, [k, ("hn", kc)])
                for kc in range(4):
                    mm(2 + b, psb[2 + b][:], ov[:, kc, :], rhs_t[:, kc, :], kc == 0, kc == 3, [k, (rhs_name, kc)])
                act(sg[:, b, :], psb[b][:], AF.Sigmoid, [("ps", b)], [("sg", b)])
                if "g" in _SKIP and first:
                    tt(yb[:, mc, :], psb[2 + b][:], sg[:, b, :], ALU.mult, [("ps", 2 + b), ("sg", b)], [("yb", mc)])
                    bgstep_ref[0](nbg)
                elif nbg and "g" not in _SKIP:
                    cp(tmpf[:, b, :], psb[2 + b][:], [("ps", 2 + b)], [("tmpf", b)], eng="act")
                    if first:
                        tt(yb[:, mc, :], tmpf[:, b, :], sg[:, b, :], ALU.mult, [("tmpf", b), ("sg", b)], [("yb", mc)],
                           eng="pool")
                    else:
                        tt(tmpf[:, b, :], tmpf[:, b, :], sg[:, b, :], ALU.mult, [("tmpf", b), ("sg", b)],
                           [("tmpf", b)], eng="pool")
                        tt(yb[:, mc, :], yb[:, mc, :], tmpf[:, b, :], ALU.add, [("yb", mc), ("tmpf", b)],
                           [("yb", mc)], eng="pool")
                    bgstep_ref[0](nbg)
                else:
                    tt(tmpf[:, b, :], psb[2 + b][:], sg[:, b, :], ALU.mult, [("ps", 2 + b), ("sg", b)], [("tmpf", b)])
                    tt(yb[:, mc, :], yb[:, mc, :], tmpf[:, b, :], ALU.add, [("yb", mc), ("tmpf", b)], [("yb", mc)],
                       eng="pool")
            item(f, wname, mc)

    bgstep_ref = [lambda n: None]

    def proj2_norm(wname, gname, dst_fn, dst_keys):
        def f(s, k):
            sv = s[:, 0:2048].rearrange("p (s c m) -> p s c m", s=2, c=8)
            for m in range(2):
                for kc in range(8):
                    mm(m, psb[m][:], sv[:, m, kc, :], hn[:, kc, :], kc == 0, kc == 7, [k, ("hn", kc)])
                cp(tmpf[:, m, :], psb[m][:], [("ps", m)], [("tmpf", m)], eng="act")
            rms_stats([tmpf[:, m, :] for m in range(2)], [("tmpf", m) for m in range(2)], TBK, 1.0 / 256)
            for m in range(2):
                stt(dst_fn(m), tmpf[:, m, :], C(gname, m), rstd[:], ALU.mult, ALU.mult,
                    [("tmpf", m), ("cst",), ("rstd",)], [dst_keys[m]])
        item(f, wname, 0)

    def mixer(seq, qb):
        t0 = qb * TBK
        nkc = 4 * (qb + 1)
        qT, oT, zT = X4[0], X4[1], X4[2]
        qmT, omT = X4[2], X4[1]
        maskT = hT

        item(lambda s, k: prenorm("mpre"))
        proj2_norm("wcq", "qng", lambda m: cqT[:, m, :], [("cqT", 0), ("cqT", 1)])
        proj2_norm("wckv", "kvng", lambda m: ckvT[:, m, t0:t0 + TBK], [("ckvT", m, qb) for m in range(2)])

        def f_tok(s, k):
            for tl in range(4):
                for m in range(2):
                    tr(psT[:, (tl % 2) * 256 + m * 128:(tl % 2) * 256 + (m + 1) * 128],
                       ckvT[:, m, t0 + tl * 128: t0 + (tl + 1) * 128], [("ckvT", m, qb)])
                cp(ckvtok[:, 4 * qb + tl, :], psT[:, (tl % 2) * 256:(tl % 2) * 256 + 256], [("ps", 7)],
                   [("ckvtok", 4 * qb + tl)])
        item(f_tok)

        def f_kidx(s, k):
            kv = s[:, 0:1024].rearrange("p (c m) -> p c m", c=8)
            wv = s[:, 1024:1056].rearrange("p (c m) -> p c m", c=8)
            if "m" in _SKIP:
                return
            for kc in range(8):
                mm(0, psb[0][:], kv[:, kc, :], hn[:, kc, :], kc == 0, kc == 7, [k, ("hn", kc)])
            if "c" in _SKIP:
                return
            cp(tmpf[:, 0, :], psb[0][:], [("ps", 0)], [("tmpf", 0)], eng="act")
            cp(sq[:, 0, :], tmpf[:, 0, :], [("tmpf", 0)], [("sq", 0)], eng="dve")
            act(sq[:, 1, :], tmpf[:, 0, :], AF.Square, [("tmpf", 0)], [("sq", 1)])
            mm(1, psb[1][:], onesb[:], sq[:, 0, :], True, True, [("sq", 0), ("onesb",)])
            mm(2, psb[2][:], onesb[:], sq[:, 1, :], True, True, [("sq", 1), ("onesb",)])
            if "l" not in _SKIP:
                ln_apply64(psb[1][0:64, :], ("ps", 1), psb[2][0:64, :], ("ps", 2))
            if "w" in _SKIP:
                return
            for qt in range(4):
                for kc in range(8):
                    mm(3, psb[3][:, qt * 4:(qt + 1) * 4], hn[:, kc, qt * 128:(qt + 1) * 128], wv[:, kc, :],
                       kc == 0, kc == 7, [k, ("hn", kc)])
            tsc(widx[:].rearrange("p a b -> p (a b)"), psb[3][:, 0:16], 0.5, ALU.mult, [("ps", 3)], [("widx",)])
        item(f_kidx, "wkidx", 0)

        def ln_apply64(psm, kmean, psv, kvar):
            P_ = slice(0, 64)
            act(tmpf[P_, 1, :], psm, AF.Copy, [kmean], [("tmpf", 1)], scale=1.0 / 64)
            tt(lnt[P_, :], tmpf[P_, 1, :], tmpf[P_, 1, :], ALU.mult, [("tmpf", 1)], [("lnt",)])
            stt(lnt[P_, :], psv, 1.0 / 64, lnt[P_, :], ALU.mult, ALU.subtract, [kvar, ("lnt",)], [("lnt",)])
            act(lnt[P_, :], lnt[P_, :], AF.Ln, [("lnt",), ("epsc",)], [("lnt",)], bias=epsc[P_, :])
            act(rstd[P_, :], lnt[P_, :], AF.Exp, [("lnt",)], [("rstd",)], scale=-0.5)
            tt(tmpf[P_, 0, :], tmpf[P_, 0, :], tmpf[P_, 1, :], ALU.subtract, [("tmpf", 0), ("tmpf", 1)], [("tmpf", 0)])
            stt(tmpf[P_, 0, :], tmpf[P_, 0, :], cst[P_, CO["ilng"]:CO["ilng"] + 1], rstd[P_, :], ALU.mult, ALU.mult,
                [("tmpf", 0), ("cst",), ("rstd",)], [("tmpf", 0)])
            tsc(kidxT[P_, t0:t0 + TBK], tmpf[P_, 0, :], cst[P_, CO["ilnb"]:CO["ilnb"] + 1], ALU.add,
                [("tmpf", 0), ("cst",)], [("kidxT", qb)])

        def f_q(s, k):
            sv = s[:, 0:1024].rearrange("p (m c n) -> p m c n", m=4, c=2)
            for m in range(4):
                b = m % 2
                for kc in range(2):
                    mm(b, psb[b][:], sv[:, m, kc, :], cqT[:, kc, :], kc == 0, kc == 1, [k, ("cqT", kc)])
                cp(qT[:, m, :], psb[b][:], [("ps", b)], [("x40", m)], eng="act" if m % 2 else "dve")
        item(f_q, "wuq", 0)

        def f_qi(s, k):
            sv = s[:, 0:1024].rearrange("p (h c n) -> p h c n", h=4, c=2)
            for h in range(4):
                b = 2 + h % 2
                for kc in range(2):
                    mm(b, psb[b][:], sv[:, h, kc, :], cqT[:, kc, :], kc == 0, kc == 1, [k, ("cqT", kc)])
                act(qidxT[:, h, :], psb[b][:], AF.Copy, [("ps", b)], [("qidxT", h)], scale=0.125)
        item(f_qi, "widxq", 0)

        def sck(qt):
            return [("SC", qt % 2, kb) for kb in range(qb + 1)]

        def scores(qt):
            sl = qt % 2
            gq = 4 * qb + qt
            SCq = SCs[sl]
            for h in range(4):
                tsc(wdiag[:, sl, h, :], identb[:], widx[:, qt, h:h + 1], ALU.mult, [("identb",), ("widx",)],
                    [("wdiag", sl)], eng="pool", s2=0.0, op1=ALU.add)
            for kb in range(qb + 1):
                nk = TBK if kb < qb else (qt + 1) * 128
                c0 = kb * TBK
                pb = 5 + (kb % 2)
                for h in range(4):
                    mm(h, psb[h][:, :nk], qidxT[:, h, qt * 128:(qt + 1) * 128], kidxT[:, c0:c0 + nk], True, True,
                       [("qidxT", h), ("kidxT", kb)])
                    if h % 2 == 0:
                        act(rl[:, h, :nk], psb[h][:, :nk], AF.Relu, [("ps", h)], [("rl", h)])
                    else:
                        tsc(rl[:, h, :nk], psb[h][:, :nk], 0.0, ALU.max, [("ps", h)], [("rl", h)])
                for h in range(4):
                    mm(pb, psb[pb][:, :nk], wdiag[:, sl, h, :], rl[:, h, :nk], h == 0, h == 3,
                       [("wdiag", sl), ("rl", h)])
                if kb < qb:
                    cp(SCq[:, c0:c0 + nk], psb[pb][:, :nk], [("ps", pb)], [("SC", sl, kb)], eng="dve")
                else:
                    if nk > 128:
                        cp(SCq[:, c0:c0 + nk - 128], psb[pb][:, :nk - 128], [("ps", pb)], [("SC", sl, kb)], eng="dve")
                    tt(SCq[:, c0 + nk - 128:c0 + nk], psb[pb][:, nk - 128:nk], causal[:], ALU.add,
                       [("ps", pb), ("causal",)], [("SC", sl, kb)])

        def bisect_gen(qts):
            tiles = [(qt % 2, 4 * qb + qt, (4 * qb + qt + 1) * 128, qt) for qt in qts]
            active = [t for t in tiles if t[1] >= GQ0]
            for (sl, gq, ncols, qt) in active:
                S.add("dve", lambda e, n=gq * 128, sl=sl: e.tensor_reduce(blo[:, sl, 0:1], SCs[sl][:, 0:n], AX.X, ALU.min),
                      reads=sck(qt), writes=[("blo", sl)])
                S.add("dve", lambda e, n=ncols, sl=sl: e.tensor_reduce(blo[:, sl, 1:2], SCs[sl][:, 0:n], AX.X, ALU.max),
                      reads=sck(qt), writes=[("blo", sl)])
            for (sl, gq, ncols, qt) in active:
                tt(bw0[:, sl:sl + 1], blo[:, sl, 1:2], blo[:, sl, 0:1], ALU.subtract, [("blo", sl)], [("bw0", sl)])
            for (sl, gq, ncols, qt) in active:
                tsc(BH[:, sl, :], C("pw", 0, NBIS), bw0[:, sl:sl + 1], ALU.mult, [("cst",), ("bw0", sl)], [("BH", sl)])
                tsc(bmid[:, sl:sl + 1], bw0[:, sl:sl + 1], 0.5, ALU.mult, [("bw0", sl), ("blo", sl)], [("bmid", sl)],
                    s2=blo[:, sl, 0:1], op1=ALU.add)
            for (sl, gq, ncols, qt) in active:
                tsc(BH2[:, sl, :], BH[:, sl, :], 2.0, ALU.mult, [("BH", sl)], [("BH2", sl)])
            yield
            for it in range(NBIS):
                for (sl, gq, ncols, qt) in active:
                    tsc(maskAs[sl][:, :ncols], SCs[sl][:, :ncols], bmid[:, sl:sl + 1], ALU.is_ge,
                        sck(qt) + [("bmid", sl)], [("maskA", sl), ("bcnt", sl)], s2=-(TOPK - 0.5), op1=ALU.add,
                        accum=bcnt[:, sl:sl + 1])
                for (sl, gq, ncols, qt) in active:
                    tsc(bu[:, sl:sl + 1], bcnt[:, sl:sl + 1], 0.0, ALU.is_gt, [("bcnt", sl), ("BH2", sl)], [("bu", sl)],
                        s2=BH2[:, sl, it:it + 1], op1=ALU.mult)
                for (sl, gq, ncols, qt) in active:
                    hh = BH if it < NBIS - 1 else BH2
                    stt(bmid[:, sl:sl + 1], bmid[:, sl:sl + 1], hh[:, sl, it:it + 1], bu[:, sl:sl + 1], ALU.subtract,
                        ALU.add, [("bmid", sl), ("BH", sl), ("BH2", sl), ("bu", sl)], [("bmid", sl)])
                yield
            for (sl, gq, ncols, qt) in tiles:
                if gq >= GQ0:
                    tsc(maskAs[sl][:, :ncols], SCs[sl][:, :ncols], bmid[:, sl:sl + 1], ALU.is_ge,
                        sck(qt) + [("bmid", sl)], [("maskA", sl)])
                else:
                    tsc(maskAs[sl][:, :ncols], SCs[sl][:, :ncols], -1e29, ALU.is_ge, sck(qt), [("maskA", sl)])

        bg = [None]

        def bgstart(qts):
            bg[0] = bisect_gen(qts)

        def bgstep(n=1):
            for _ in range(n):
                if bg[0] is None:
                    return
                try:
                    next(bg[0])
                except StopIteration:
                    bg[0] = None

        def bgdrain():
            while bg[0] is not None:
                bgstep()

        def mask_transposes(qt):
            sl = qt % 2
            gq = 4 * qb + qt
            for k0 in range(0, gq + 1, 4):
                n4 = min(4, gq + 1 - k0)
                half = ((k0 // 4) % 2) * 512
                for i in range(n4):
                    tr(psT[:, half + i * 128: half + (i + 1) * 128], maskAs[sl][:, (k0 + i) * 128:(k0 + i + 1) * 128],
                       [("maskA", sl)])
                cp(maskT[:, k0:k0 + n4, qt * 128:(qt + 1) * 128],
                   psT[:, half:half + n4 * 128].rearrange("p (a b) -> p a b", a=n4), [("ps", 7)],
                   [("hT", k0 + i) for i in range(n4)], eng="act")
            for kc in range(gq + 1, nkc):
                memset(maskT[:, kc, qt * 128:(qt + 1) * 128], 0.0, [("hT", kc)], eng="pool")

        def f_idx01(s, k):
            bgstep_ref[0] = bgstep
            scores(0)
            scores(1)
            bgstart([0, 1])
        item(f_idx01)

        for c in range(4):
            def f(s, k, c=c):
                sv = s[:, 0:2048].rearrange("p (s c m) -> p s c m", s=2, c=8)
                b = gi[0] % 2
                gi[0] += 1
                for kc in range(8):
                    mm(b, psb[b][:], sv[:, 0, kc, :], hn[:, kc, :], kc == 0, kc == 7, [k, ("hn", kc)])
                for kc in range(8):
                    mm(2 + b, psb[2 + b][:], sv[:, 1, kc, :], hn[:, kc, :], kc == 0, kc == 7, [k, ("hn", kc)])
                act(sg[:, b, :], psb[2 + b][:], AF.Sigmoid, [("ps", 2 + b)], [("sg", b)])
                tt(uT[:, c, 32:32 + TBK], psb[b][:], sg[:, b, :], ALU.mult, [("ps", b), ("sg", b)], [("uT", c)])
                bgstep(1)
            item(f, "wu", c)

        def f_conv(s, k):
            for c in range(4):
                for j in range(31):
                    if j % 2 == 0 or "d" in _SKIP:
                        tsc(cdiag[:, j, :], identb[:], C("wdw", j * 4 + c), ALU.mult, [("identb",), ("cst",)],
                            [("cdiag", j)], eng="pool", s2=0.0, op1=ALU.add)
                    else:
                        act(cdiag[:, j, :], identb[:], AF.Identity, [("identb",), ("cst",)], [("cdiag", j)],
                            scale=C("wdw", j * 4 + c))
                b = 5 + c % 2
                for j in range(31):
                    mm(b, psb[b][:], cdiag[:, j, :], uT[:, c, 2 + j:2 + j + TBK], j == 0, j == 30,
                       [("cdiag", j), ("uT", c)])
                cp(uT[:, c, 0:32], uT[:, c, TBK:TBK + 32], [("uT", c)], [("uT", c)], eng="pool")
                act(zT[:, c, :], psb[b][:], AF.Identity, [("ps", b), ("cst",)], [("x42", c)], bias=C("bdw", c))
                q = sqi[0] % 2
                sqi[0] += 1
                act(sq[:, q, :], zT[:, c, :], AF.Square, [("x42", c)], [("sq", q)])
                mm(0, psb[0][:], onesb[:], zT[:, c, :], c == 0, c == 3, [("x42", c), ("onesb",)])
                mm(1, psb[1][:], onesb[:], sq[:, q, :], c == 0, c == 3, [("sq", q), ("onesb",)])
                bgstep(2)
            act(tmpf[:, 1, :], psb[0][:], AF.Copy, [("ps", 0)], [("tmpf", 1)], scale=1.0 / 512)
            tt(lnt[:], tmpf[:, 1, :], tmpf[:, 1, :], ALU.mult, [("tmpf", 1)], [("lnt",)])
            stt(lnt[:], psb[1][:], 1.0 / 512, lnt[:], ALU.mult, ALU.subtract, [("ps", 1), ("lnt",)], [("lnt",)])
            act(lnt[:], lnt[:], AF.Ln, [("lnt",), ("epsc",)], [("lnt",)], bias=epsc[:])
            act(rstd[:], lnt[:], AF.Exp, [("lnt",)], [("rstd",)], scale=-0.5)
            for c in range(4):
                tt(tmpf[:, 0, :], zT[:, c, :], tmpf[:, 1, :], ALU.subtract, [("x42", c), ("tmpf", 1)],
                   [("tmpf", 0)])
                stt(tmpf[:, 0, :], tmpf[:, 0, :], C("clng", c), rstd[:], ALU.mult, ALU.mult,
                    [("tmpf", 0), ("cst",), ("rstd",)], [("tmpf", 0)])
                act(zT[:, c, :], tmpf[:, 0, :], AF.Silu, [("tmpf", 0), ("cst",)], [("x42", c)], bias=C("clnb", c))
        item(f_conv)
        gated_out("wgb", zT, "x42", True, nbg=1)

        def f_idx23(s, k):
            bgdrain()
            mask_transposes(0)
            mask_transposes(1)
            scores(2)
            scores(3)
            bgstart([2, 3])
        item(f_idx23)

        for g in range(2):
            def f(s, k, g=g):
                sv = s[:, 0:2048].rearrange("p (s c m) -> p s c m", s=2, c=8)
                for m in range(2):
                    b = m
                    for kc in range(8):
                        mm(b, psb[b][:], sv[:, m, kc, :], hn[:, kc, :], kc == 0, kc == 7, [k, ("hn", kc)])
                    cp(qmT[:, 2 * g + m, :], psb[b][:], [("ps", b)], [("x42", 2 * g + m)], eng="act")
                bgstep(2)
            item(f, "wqm", g)

        pti = [0]

        def f_mattn(s, k):
            for h in range(4):
                hb = h % 2
                for mc2 in range(2):
                    mm(mc2, psb[mc2][:], memkT[:, h, mc2 * 128:(mc2 + 1) * 128], qmT[:, h, :], True, True,
                       [("memkT", h), ("x42", h)])
                    p = pti[0] % 3
                    pti[0] += 1
                    act(PT[:, p, :], psb[mc2][:], AF.Exp, [("ps", mc2)], [("PT", p)], scale=128.0 ** -0.5)
                    mm(2 + hb, psb[2 + hb][:], memv[:, mc2, h * 128:(h + 1) * 128], PT[:, p, :], mc2 == 0, mc2 == 1,
                       [("memv", mc2), ("PT", p)])
                    mm(5 + hb, psb[5 + hb][:], onesb[:], PT[:, p, :], mc2 == 0, mc2 == 1, [("onesb",), ("PT", p)])
                act(lnt[:], psb[5 + hb][:], AF.Ln, [("ps", 5 + hb)], [("lnt",)])
                act(rinv[:, hb, :], lnt[:], AF.Exp, [("lnt",)], [("rinv", hb)], scale=-1.0)
                tt(omT[:, h, :], psb[2 + hb][:], rinv[:, hb, :], ALU.mult, [("ps", 2 + hb), ("rinv", hb)], [("x41", h)])
                bgstep(1)
        item(f_mattn)
        gated_out("wgm", omT, "x41", False, nbg=2)

        def f_idx_end(s, k):
            bgdrain()
            mask_transposes(2)
            mask_transposes(3)
        item(f_idx_end)

        def f_attn(s, k):
            steps = [(h, kc) for h in range(8) for kc in range(nkc)]

            def emit_qlat(h, ccs=(0, 1)):
                hb = h % 2
                for cc in ccs:
                    mm(4, psb[4][:], wukr[:, h, cc, :], qT[:, h // 2, :], True, True, [("wukr",), ("x40", h // 2)])
                    act(qlat[:, hb, cc, :], psb[4][:], AF.Copy, [("ps", 4)], [("qlat", hb, cc)], scale=0.125)

            def q0(kc):
                return max(0, kc - 4 * qb) * 128

            def emit_S(i):
                h, kc = steps[i]
                hb, sbk, p = h % 2, i % 2, i % 3
                c0 = q0(kc)
                for cc in range(2):
                    mm(sbk, psb[sbk][:, c0:], ckvT[:, cc, kc * 128:(kc + 1) * 128], qlat[:, hb, cc, c0:], cc == 0,
                       cc == 1, [("ckvT", cc, kc // 4), ("qlat", hb, cc)])
                act(PT[:, p, c0:], psb[sbk][:, c0:], AF.Exp, [("ps", sbk)], [("PT", p)])
                tt(PT[:, p, c0:], PT[:, p, c0:], maskT[:, kc, c0:], ALU.mult, [("PT", p), ("hT", kc)], [("PT", p)])

            def emit_PV(i):
                h, kc = steps[i]
                p = i % 3
                c0 = q0(kc)
                for cc in range(2):
                    mm(2 + cc, psb[2 + cc][:, c0:], ckvtok[:, kc, cc * 128:(cc + 1) * 128], PT[:, p, c0:], kc == 0,
                       kc == nkc - 1, [("ckvtok", kc), ("PT", p)])
                mm(5, psb[5][:, c0:], onesb[:], PT[:, p, c0:], kc == 0, kc == nkc - 1, [("onesb",), ("PT", p)])

            def fin_elem(h):
                hb = h % 2
                cp(tmpf[:, 0, :], psb[2][:], [("ps", 2)], [("tmpf", 0)], eng="act")
                cp(tmpf[:, 1, :], psb[3][:], [("ps", 3)], [("tmpf", 1)], eng="dve")
                act(lnt[:], psb[5][:], AF.Ln, [("ps", 5)], [("lnt",)])
                act(rinv[:, hb, :], lnt[:], AF.Exp, [("lnt",)], [("rinv", hb)], scale=-1.0)
                tt(olat[:, hb, 0, :], tmpf[:, 0, :], rinv[:, hb, :], ALU.mult, [("tmpf", 0), ("rinv", hb)],
                   [("olat", hb, 0)])
                tt(olat[:, hb, 1, :], tmpf[:, 1, :], rinv[:, hb, :], ALU.mult, [("tmpf", 1), ("rinv", hb)],
                   [("olat", hb, 1)], eng="pool")

            def fin_pe(h):
                hb = h % 2
                for cc in range(2):
                    mm(6, psb[6][:], wuvr[:, h, cc, :], olat[:, hb, cc, :], hb == 0 and cc == 0, hb == 1 and cc == 1,
                       [("wuvr",), ("olat", hb, cc)])
                if hb == 1:
                    cp(oT[:, h // 2, :], psb[6][:], [("ps", 6)], [("x41", h // 2)], eng="act")

            emit_qlat(0)
            emit_S(0)
            pend = []
            for i in range(len(steps)):
                h, kc = steps[i]
                if kc < 2 and h + 1 < 8:
                    emit_qlat(h + 1, (kc,))
                if "p" in _SKIP:
                    if i > 0:
                        emit_S(i)
                elif i + 1 < len(steps):
                    emit_S(i + 1)
                emit_PV(i)
                for pe_ in pend:
                    pe_[1] -= 1
                while pend and pend[0][1] <= 0:
                    fin_pe(pend.pop(0)[0])
                if kc == nkc - 1:
                    fin_elem(h)
                    pend.append([h, 3])
            for pe_ in pend:
                fin_pe(pe_[0])
        item(f_attn)
        gated_out("wga", oT, "x41", False, nbg=0)

        def f_mb(s, k):
            for kc in range(8):
                cp(hn[:, kc, :], yb[:, kc, :], [("yb", kc)], [("hn", kc)], eng="act" if kc % 2 else "dve")
        item(f_mb)
        for g in range(4):
            def f(s, k, g=g):
                sv = s[:, 0:2048].rearrange("p (s c m) -> p s c m", s=2, c=8)
                for m in range(2):
                    mc = 2 * g + m
                    b = 5 + m
                    for kc in range(8):
                        mm(b, psb[b][:], sv[:, m, kc, :], hn[:, kc, :], kc == 0, kc == 7, [k, ("hn", kc)])
                    flush_stats()
                    q = sqi[0] % 2
                    sqi[0] += 1
                    act(sq[:, q, :], psb[b][:], AF.Square, [("ps", b)], [("sq", q)])
                    cp(yb[:, mc, :], psb[b][:], [("ps", b)], [("yb", mc)], eng="act")
                    pend_stats.append((q, mc))
            item(f, "wout", g)

        def f_mpost(s, k):
            flush_stats()
            postnorm_residual(lambda mc: C("mpost", mc), ("cst",))
        item(f_mpost)

    def mem_prep(seq):
        sem = new_dsem()

        def f_load(s, k):
            for kc in range(8):
                dma_plain(SCs[0][:, kc * NMEM:(kc + 1) * NMEM], memT[seq, kc * 128:(kc + 1) * 128, :], [],
                          [("SC", 0, kc // 2)], sem)
            rms_stats([SCs[0][:, kc * NMEM:(kc + 1) * NMEM] for kc in range(8)], [("SC", 0, kc // 2) for kc in range(8)],
                      NMEM, 1.0 / D)
            mn = X4[2][:].rearrange("p a b -> p (a b)")
            for kc in range(8):
                stt(mn[:, kc * NMEM:(kc + 1) * NMEM], SCs[0][:, kc * NMEM:(kc + 1) * NMEM], C("memg", kc),
                    rstd[:, :NMEM], ALU.mult, ALU.mult, [("SC", 0, kc // 2), ("cst",), ("rstd",)], [("x42", kc // 2)])
        item(f_load)
        for g in range(2):
            def f(s, k, g=g):
                sv = s[:, 0:2048].rearrange("p (s c m) -> p s c m", s=2, c=8)
                mn = X4[2][:].rearrange("p a b -> p (a b)")
                for m in range(2):
                    b = m
                    for kc in range(8):
                        mm(b, psb[b][:, :NMEM], sv[:, m, kc, :], mn[:, kc * NMEM:(kc + 1) * NMEM], kc == 0, kc == 7,
                           [k, ("x42", kc // 2)])
                    cp(memkT[:, 2 * g + m, :], psb[b][:, :NMEM], [("ps", b)], [("memkT", 2 * g + m)],
                       eng="act" if m else "dve")
            item(f, "wmk", g)
        for g in range(2):
            def f(s, k, g=g):
                sv = s[:, 0:2048].rearrange("p (c n) -> p c n", c=4)
                mn = X4[2][:].rearrange("p a b -> p (a b)")
                for mc2 in range(2):
                    for kl in range(4):
                        kc = 4 * g + kl
                        mm(2 + mc2, psb[2 + mc2][:], mn[:, kc * NMEM + mc2 * 128: kc * NMEM + (mc2 + 1) * 128],
                           sv[:, kl, :], kc == 0, kc == 7, [k, ("x42", kc // 2)])
                    if g == 1:
                        cp(memv[:, mc2, :], psb[2 + mc2][:], [("ps", 2 + mc2)], [("memv", mc2)],
                           eng="act" if mc2 else "dve")
            item(f, "wmv", g)

    out_sems = []
    for seq in range(NSEQ):
        mem_prep(seq)
        if seq > 0:
            item(lambda s, k: memset(uT[:], 0.0, [("uT", c) for c in range(4)], eng="dve"))
        for qb in range(NQB):
            t0 = qb * TBK
            semx = new_dsem()

            def f_x(s, k, seq=seq, t0=t0, semx=semx):
                for kc in range(8):
                    dma_plain(xres[:, kc, :], xT[seq, kc * 128:(kc + 1) * 128, t0:t0 + TBK], [], [("xres", kc)], semx)
            item(f_x)
            ffn("w1gu", "w1dn", "f1pre", 0)
            mixer(seq, qb)
            ffn("w2gu", "w2dn", "f2pre", 8)
            semo = new_dsem()
            out_sems.append(semo)

            def f_o(s, k, seq=seq, t0=t0, semo=semo):
                for kc in range(8):
                    dma_plain(outT[seq, kc * 128:(kc + 1) * 128, t0:t0 + TBK], xres[:, kc, :], [("xres", kc)],
                              [("out", seq, t0, kc)], semo)
            item(f_o)

    loads = [(i, it) for i, it in enumerate(items) if it[0] is not None]
    slot_of = {}
    nl = [0]

    def issue_load():
        li = nl[0]
        if li >= len(loads):
            return
        i, (w, g, _) = loads[li]
        sl = li % NBUF
        n = weight_specs()[w][1]
        dma_cast(ring[sl][:, 0:n], wd[w][g], [], [("ring", sl)], ring_sem[sl])
        slot_of[i] = sl
        nl[0] += 1

    import os as _os
    _n = int(_os.environ.get("KDBG_N", "0"))
    if _n:
        items[:] = items[:_n]
        loads[:] = [(i, it) for i, it in enumerate(items) if it[0] is not None]
    for _ in range(NBUF - 1):
        issue_load()
    for i, (w, g, fn) in enumerate(items):
        if w is not None:
            issue_load()
            sl = slot_of[i]
            fn(ring[sl], ("ring", sl))
        else:
            fn(None, None)

    fin_reads = []
    for seq in range(NSEQ):
        for qb in range(NQB):
            for kc in range(8):
                fin_reads.append(("out", seq, qb * TBK, kc))
    S.add("sp", lambda e: e.nop(), reads=fin_reads, writes=[])

    S.finalize()
    with ExitStack() as es2:
        for k in set(op.skey for op in S.ops if op.sig):
            sems[k] = es2.enter_context(nc.semaphore("s_%s_%s" % (k[0], k[1])))
        block = es2.enter_context(nc.Block())

        @block.tensor
        def _(e):
            S.emit("pe", e, sems)

        @block.scalar
        def _(e):
            S.emit("act", e, sems)

        @block.vector
        def _(e):
            S.emit("dve", e, sems)

        @block.gpsimd
        def _(e):
            S.emit("pool", e, sems)

        @block.sync
        def _(e):
            S.emit("sp", e, sems)
    es.close()
    return nc


def lhsT_tiles(W, mchunk=128):
    K, M = W.shape
    return np.ascontiguousarray(W.reshape(K // 128, 128, M // mchunk, mchunk).transpose(2, 1, 0, 3))


def col_tile(v):
    n = v.shape[0]
    if n < 128:
        o = np.zeros((128, 1), np.float32)
        o[:n, 0] = v
        return o
    return np.ascontiguousarray(v.reshape(n // 128, 128).T)


def prep_weights(p):
    f = np.float32
    w = {}
    for i, gu, dn in ((1, p["ffn1_w_gu"][0], p["ffn1_w_down"][0]), (2, p["ffn2_w_gu"][0], p["ffn2_w_down"][0])):
        g = lhsT_tiles(gu[:, :DFF])
        u = lhsT_tiles(gu[:, DFF:])
        w["w%dgu" % i] = np.stack([g, u], axis=2).reshape(NJ, 128, 2048)
        w["w%ddn" % i] = lhsT_tiles(dn).reshape(8, 128, DFF)
    win = p["w_in"][0]
    o_cq, o_ckv, o_ki, o_wi, o_u, o_qm, o_g = 0, 256, 512, 576, 580, 1604, 2116
    w["wcq"] = lhsT_tiles(win[:, o_cq:o_cq + 256]).transpose(1, 0, 2, 3).reshape(1, 128, 2048)
    w["wckv"] = lhsT_tiles(win[:, o_ckv:o_ckv + 256]).transpose(1, 0, 2, 3).reshape(1, 128, 2048)
    kip = np.zeros((D, 128), f)
    kip[:, :64] = win[:, o_ki:o_ki + 64]
    ki = lhsT_tiles(kip)[0].reshape(128, 1024)
    wi = lhsT_tiles(win[:, o_wi:o_wi + 4], 4)[0].reshape(128, 32)
    w["wkidx"] = np.concatenate([ki, wi], axis=1)[None]
    w["wuq"] = lhsT_tiles(p["w_uq"][0]).transpose(1, 0, 2, 3).reshape(1, 128, 1024)
    wiq = np.zeros((256, 4, 128), f)
    wiq[:, :, :64] = p["w_idx_q"][0].reshape(256, 4, 64)
    w["widxq"] = lhsT_tiles(wiq.reshape(256, 512)).transpose(1, 0, 2, 3).reshape(1, 128, 1024)
    wuk = p["w_uk"][0]
    wuv = p["w_uv"][0]
    uk = np.zeros((128, 8, 2, 128), f)
    uv = np.zeros((128, 8, 2, 128), f)
    for h in range(8):
        r0 = (h % 2) * 64
        for cc in range(2):
            uk[r0:r0 + 64, h, cc, :] = wuk[cc * 128:(cc + 1) * 128, h, :].T
            uv[:, h, cc, r0:r0 + 64] = wuv[cc * 128:(cc + 1) * 128, h, :]
    w["wuk"] = uk.reshape(1, 128, 2048)
    w["wuv"] = uv.reshape(1, 128, 2048)
    for bi, (nm, wo) in enumerate((("wga", "w_dsa_o"), ("wgb", "w_conv_out"), ("wgm", "w_mem_o"))):
        gt = lhsT_tiles(win[:, o_g + bi * D:o_g + (bi + 1) * D]).reshape(8, 128, 1024)
        ot = lhsT_tiles(p[wo][0]).reshape(8, 128, 512)
        w[nm] = np.concatenate([gt, ot], axis=2)
    ua = lhsT_tiles(win[:, o_u:o_u + 512])
    ug = lhsT_tiles(win[:, o_u + 512:o_u + 1024])
    w["wu"] = np.stack([ua, ug], axis=2).reshape(4, 128, 2048)
    qm = lhsT_tiles(win[:, o_qm:o_qm + 512])
    w["wqm"] = qm.reshape(2, 2, 128, 8, 128).transpose(0, 2, 1, 3, 4).reshape(2, 128, 2048)
    mkv = p["w_mem_kv"][0]
    mk = lhsT_tiles(mkv[:, :512])
    w["wmk"] = mk.reshape(2, 2, 128, 8, 128).transpose(0, 2, 1, 3, 4).reshape(2, 128, 2048)
    mv = mkv[:, 512:].reshape(8, 128, 512)
    w["wmv"] = mv.reshape(2, 4, 128, 512).transpose(0, 2, 1, 3).reshape(2, 128, 2048)
    wo = lhsT_tiles(p["w_out"][0])
    w["wout"] = wo.reshape(4, 2, 128, 8, 128).transpose(0, 2, 1, 3, 4).reshape(4, 128, 2048)
    CO, NC_ = cst_layout()
    cst = np.zeros((128, NC_), f)

    def put(name, v):
        t = col_tile(np.asarray(v, f))
        cst[:, CO[name]:CO[name] + t.shape[1]] = t
    put("f1pre", p["ffn1_pre_g"][0]); put("f1post", p["ffn1_post_g"][0])
    put("mpre", p["mix_pre_g"][0]); put("mpost", p["mix_post_g"][0])
    put("f2pre", p["ffn2_pre_g"][0]); put("f2post", p["ffn2_post_g"][0])
    put("memg", p["mem_norm_g"][0]); put("qng", p["q_norm_g"][0]); put("kvng", p["kv_norm_g"][0])
    put("ilng", p["idx_ln_g"][0]); put("ilnb", p["idx_ln_b"][0])
    put("bdw", p["b_dw"][0]); put("clng", p["conv_ln_g"][0]); put("clnb", p["conv_ln_b"][0])
    wdw = p["w_dw"][0]
    cst[:, CO["wdw"]:CO["wdw"] + 124] = wdw.reshape(31, 4, 128).transpose(2, 0, 1).reshape(128, 124)
    cst[:, CO["pw"]:CO["pw"] + NBIS] = np.float32(2.0) ** -(np.arange(NBIS, dtype=f) + 2)
    w["cst"] = cst
    w["ident"] = np.eye(128, dtype=f)
    cm = np.zeros((128, 128), f)
    cm[np.triu_indices(128, 1)] = -1e30
    w["causal"] = cm
    return {k: np.ascontiguousarray(v, dtype=f) for k, v in w.items()}


_NC_CACHE = {}


def run(inputs, T, B, n_cores, runner=None):
    p = {k: np.asarray(v) for k, v in inputs.items()}
    NSEQ = B // n_cores
    w = prep_weights(p)
    x = np.asarray(p["x"], np.float32)
    mem = np.asarray(p["mem"], np.float32)
    xT = np.ascontiguousarray(x.transpose(0, 2, 1))
    mT = np.ascontiguousarray(mem.transpose(0, 2, 1))
    key = (T, NSEQ)
    if key not in _NC_CACHE:
        _NC_CACHE[key] = build_nc(T, NSEQ)
    nc = _NC_CACHE[key]
    in_maps = []
    for c in range(n_cores):
        m = dict(w)
        m["xT"] = xT[c * NSEQ:(c + 1) * NSEQ]
        m["memT"] = mT[c * NSEQ:(c + 1) * NSEQ]
        in_maps.append(m)
    if runner is None:
        res = run_bass_kernel_spmd(nc, in_maps, core_ids=list(range(n_cores)))
        outs = [r["outT"] for r in res.results]
    else:
        outs = runner(nc, in_maps)
    oT = np.concatenate(outs, axis=0)
    return np.ascontiguousarray(oT.transpose(0, 2, 1)).astype(np.float32)


def kernel(**inputs):
    return run(inputs, 2048, 16, 8)
```
